# Optimizing a Trainium2 kernel written in Bass

```python
import math
import jax, jax.numpy as jnp
from jax import lax
import numpy as np

D_MODEL = 2048
BATCH = 4
SEQ = 2048
DEPTH = 2

GRID_W = 64
CTX_LEN = 256
Q_BLOCK = 128
ROPE_THETA = 10000.0
NORM_EPS = 1e-6
D_MIX = D_MODEL

RWKV_HEAD = 64
RWKV_HEADS = (D_MIX // 4) // RWKV_HEAD
RWKV_W = RWKV_HEADS * RWKV_HEAD
DECAY_RANK = 64
ICLR_RANK = 64
GATE_RANK = 128
GN_EPS = 64e-5
DECAY_SCALE = math.exp(-0.5)

GQA_HEAD = 128
GQA_Q_HEADS = (D_MIX // 2) // GQA_HEAD
GQA_KV_HEADS = 2
GQA_REP = GQA_Q_HEADS // GQA_KV_HEADS

MLA_HEADS = 4
MLA_NOPE = 128
MLA_ROPE = 64
MLA_V = (D_MIX // 4) // MLA_HEADS
MLA_Q_RANK = 384
MLA_KV_RANK = 256

N_GROUPS = 4
EXPERTS_PER_GROUP = 8
N_EXPERTS = N_GROUPS * EXPERTS_PER_GROUP
TOP_K = 2
D_EXPERT = 256

RWKV_SPLITS = (RWKV_W, RWKV_W, RWKV_W, DECAY_RANK, ICLR_RANK, GATE_RANK)
GQA_SPLITS = (GQA_Q_HEADS * GQA_HEAD, GQA_KV_HEADS * GQA_HEAD, GQA_KV_HEADS * GQA_HEAD)
MLA_SPLITS = (MLA_Q_RANK, MLA_KV_RANK, MLA_ROPE)
RWKV_COLS = sum(RWKV_SPLITS)
GQA_COLS = sum(GQA_SPLITS)
MLA_COLS = sum(MLA_SPLITS)
D_IN = RWKV_COLS + GQA_COLS + MLA_COLS

kernel_name = "hybrid_rwkv7_gqa_mla_hmoe_dit"


def split_cols(x, sizes):
    idx = np.cumsum(sizes)[:-1].tolist()
    return jnp.split(x, idx, axis=-1)


def rmsnorm(x, g):
    xf = x.astype(jnp.float32)
    y = xf * lax.rsqrt(jnp.mean(xf * xf, axis=-1, keepdims=True) + NORM_EPS)
    return y.astype(x.dtype) * g


def modulate(h, shift, scale):
    return h * (1 + scale) + shift


def to_heads(p, n_heads):
    B, T, _ = p.shape
    return p.reshape(B, T, n_heads, -1).transpose(0, 2, 1, 3)


def axial_rope(x, row, col):
    n = x.shape[-1]
    half = n // 2
    quarter = half // 2
    inv = ROPE_THETA ** (-jnp.arange(quarter, dtype=jnp.float32) / quarter)

    def rot(xa, pos):
        ang = pos[:, None] * inv[None, :]
        cos = jnp.cos(ang).astype(x.dtype)
        sin = jnp.sin(ang).astype(x.dtype)
        x1, x2 = xa[..., :quarter], xa[..., quarter:]
        return jnp.concatenate([x1 * cos - x2 * sin, x1 * sin + x2 * cos], axis=-1)

    return jnp.concatenate([rot(x[..., :half], row), rot(x[..., half:], col)], axis=-1)


def sweep_attention(q, k, v, scale):
    B, G, R, Tq, dk = q.shape
    nb = Tq // Q_BLOCK
    qb = q.reshape(B, G, R, nb, Q_BLOCK, dk).transpose(3, 0, 1, 2, 4, 5)

    def one_block(qblk):
        s = jnp.einsum('bgrqd,bgkd->bgrqk', qblk, k).astype(jnp.float32) * scale
        p = jax.nn.softmax(s, axis=-1)
        return jnp.einsum('bgrqk,bgkd->bgrqd', p.astype(v.dtype), v)

    o = lax.map(one_block, qb)
    return o.transpose(1, 2, 3, 0, 4, 5).reshape(B, G, R, Tq, v.shape[-1])


def attend_heads(q, k, v, rep, scale):
    B, Hq, T, d = q.shape
    o = sweep_attention(q.reshape(B, Hq // rep, rep, T, d), k, v, scale)
    dv = v.shape[-1]
    return o.reshape(B, Hq, T, dv).transpose(0, 2, 1, 3).reshape(B, T, Hq * dv)


def token_shift(p, mu_prev, mu_next):
    prev = jnp.pad(p[:, :-1], ((0, 0), (1, 0), (0, 0)))
    nxt = jnp.pad(p[:, 1:], ((0, 0), (0, 1), (0, 0)))
    return p + mu_prev * (prev - p) + mu_next * (nxt - p)


def rwkv_prep(p, mu_prev, mu_next, w0, w_up, a0, a_up, k_k, k_a):
    z = token_shift(p, mu_prev, mu_next)
    r, k, v, wd, ad, gd = split_cols(z, RWKV_SPLITS)
    B, T, _ = p.shape
    hd = lambda t: t.reshape(B, T, RWKV_HEADS, RWKV_HEAD)
    kap = hd(k * k_k).astype(jnp.float32)
    khat = kap * lax.rsqrt(jnp.sum(kap * kap, axis=-1, keepdims=True) + 1e-12)
    wd_t = jnp.tanh(wd)
    dirs = []
    for d in range(2):
        w = jnp.exp(-DECAY_SCALE * jax.nn.sigmoid((w0[d] + wd_t @ w_up[d]).astype(jnp.float32)))
        a = jax.nn.sigmoid((a0[d] + ad @ a_up[d]).astype(jnp.float32))
        kt = k.astype(jnp.float32) * (1 + (a - 1) * k_a)
        dirs.append((hd(w), hd(a) * khat, hd(kt)))
    return hd(r), hd(v), khat, dirs, gd


def wkv_scan(s0, r, w, khat, b, v, kt, reverse, emit):
    tm = lambda t: jnp.moveaxis(t.astype(jnp.float32), 1, 0)

    def step(s, inp):
        r_t, w_t, kh_t, b_t, v_t, kt_t = inp
        sk = jnp.einsum('bhvk,bhk->bhv', s, kh_t)
        s = s * w_t[:, :, None, :] - sk[..., None] * b_t[:, :, None, :] + v_t[..., None] * kt_t[:, :, None, :]
        y = jnp.einsum('bhvk,bhk->bhv', s, r_t) if emit else None
        return s, y

    xs = (tm(r), tm(w), tm(khat), tm(b), tm(v), tm(kt))
    s_fin, ys = lax.scan(step, s0, xs, reverse=reverse)
    return s_fin, (jnp.moveaxis(ys, 0, 1) if emit else None)


def rwkv_readout(r, v, kts, ys, gd, g_up, r_k, gn_g, gn_b):
    B, T, H, K = r.shape
    y = ys[0] + ys[1]
    mu = jnp.mean(y, axis=-1, keepdims=True)
    var = jnp.mean(jnp.square(y - mu), axis=-1, keepdims=True)
    yn = (y - mu) * lax.rsqrt(var + GN_EPS) * gn_g.reshape(H, K) + gn_b.reshape(H, K)
    bonus = jnp.sum(r.astype(jnp.float32) * (kts[0] + kts[1]) * r_k.reshape(H, K), axis=-1, keepdims=True) * v.astype(jnp.float32)
    g = jax.nn.sigmoid(gd) @ g_up
    return (yn + bonus).reshape(B, T, H * K).astype(gd.dtype) * g


def rwkv_mixer(p_lat, p_ctx, mu_prev, mu_next, w0, w_up, a0, a_up, g_up, k_k, k_a, r_k, gn_g, gn_b, need_ctx):
    B = p_lat.shape[0]
    s0 = jnp.zeros((B, RWKV_HEADS, RWKV_HEAD, RWKV_HEAD), jnp.float32)
    r_c, v_c, kh_c, dirs_c, gd_c = rwkv_prep(p_ctx, mu_prev, mu_next, w0, w_up, a0, a_up, k_k, k_a)
    r_l, v_l, kh_l, dirs_l, gd_l = rwkv_prep(p_lat, mu_prev, mu_next, w0, w_up, a0, a_up, k_k, k_a)
    ys_l, ys_c = [], []
    for d in range(2):
        rev = d == 1
        w_c, b_c, kt_c = dirs_c[d]
        w_l, b_l, kt_l = dirs_l[d]
        s_c, y_c = wkv_scan(s0, r_c, w_c, kh_c, b_c, v_c, kt_c, rev, need_ctx)
        _, y_l = wkv_scan(s_c, r_l, w_l, kh_l, b_l, v_l, kt_l, rev, True)
        ys_l.append(y_l)
        ys_c.append(y_c)
    o_l = rwkv_readout(r_l, v_l, [dirs_l[0][2], dirs_l[1][2]], ys_l, gd_l, g_up, r_k, gn_g, gn_b)
    o_c = rwkv_readout(r_c, v_c, [dirs_c[0][2], dirs_c[1][2]], ys_c, gd_c, g_up, r_k, gn_g, gn_b) if need_ctx else None
    return o_l, o_c


def gqa_mixer(p_lat, p_ctx, row, col, q_norm_g, k_norm_g, need_ctx):
    def kv(p):
        _, k, v = split_cols(p, GQA_SPLITS)
        return rmsnorm(to_heads(k, GQA_KV_HEADS), k_norm_g), to_heads(v, GQA_KV_HEADS)

    def q_of(p):
        return rmsnorm(to_heads(split_cols(p, GQA_SPLITS)[0], GQA_Q_HEADS), q_norm_g)

    scale = GQA_HEAD ** -0.5
    k_c, v_c = kv(p_ctx)
    k_l, v_l = kv(p_lat)
    k_l = axial_rope(k_l, row, col)
    q_l = axial_rope(q_of(p_lat), row, col)
    o_l = attend_heads(q_l, jnp.concatenate([k_c, k_l], axis=2), jnp.concatenate([v_c, v_l], axis=2), GQA_REP, scale)
    o_c = attend_heads(q_of(p_ctx), k_c, v_c, GQA_REP, scale) if need_ctx else None
    return o_l, o_c


def mla_kv(p, kv_norm_g, w_ukv, row, col, latent):
    _, ckv, kr = split_cols(p, MLA_SPLITS)
    B, T, _ = p.shape
    kv = to_heads(rmsnorm(ckv, kv_norm_g) @ w_ukv, MLA_HEADS)
    k_nope, v = kv[..., :MLA_NOPE], kv[..., MLA_NOPE:]
    if latent:
        kr = axial_rope(kr, row, col)
    kr = jnp.broadcast_to(kr[:, None], (B, MLA_HEADS, T, MLA_ROPE))
    return jnp.concatenate([k_nope, kr], axis=-1), v


def mla_q(p, q_norm_g, w_uq, row, col, latent):
    cq = split_cols(p, MLA_SPLITS)[0]
    q = to_heads(rmsnorm(cq, q_norm_g) @ w_uq, MLA_HEADS)
    q_nope, q_rope = q[..., :MLA_NOPE], q[..., MLA_NOPE:]
    if latent:
        q_rope = axial_rope(q_rope, row, col)
    return jnp.concatenate([q_nope, q_rope], axis=-1)


def mla_mixer(p_lat, p_ctx, row, col, q_norm_g, w_uq, kv_norm_g, w_ukv, need_ctx):
    scale = (MLA_NOPE + MLA_ROPE) ** -0.5
    k_c, v_c = mla_kv(p_ctx, kv_norm_g, w_ukv, row, col, False)
    k_l, v_l = mla_kv(p_lat, kv_norm_g, w_ukv, row, col, True)
    q_l = mla_q(p_lat, q_norm_g, w_uq, row, col, True)
    o_l = attend_heads(q_l, jnp.concatenate([k_c, k_l], axis=2), jnp.concatenate([v_c, v_l], axis=2), 1, scale)
    o_c = attend_heads(mla_q(p_ctx, q_norm_g, w_uq, row, col, False), k_c, v_c, 1, scale) if need_ctx else None
    return o_l, o_c


def hier_moe(h, wg, bg, we, be, w1, w3, w2):
    N = h.shape[0]
    g_logits = (h @ wg).astype(jnp.float32) + bg
    pg = jax.nn.softmax(g_logits, axis=-1)
    oh_g = jax.nn.one_hot(jnp.argmax(g_logits, axis=-1), N_GROUPS, dtype=jnp.float32)
    p_sel = jnp.sum(pg * oh_g, axis=-1)
    e_logits = ((h @ we).astype(jnp.float32) + be).reshape(N, N_GROUPS, EXPERTS_PER_GROUP)
    pe = jax.nn.softmax(jnp.einsum('nge,ng->ne', e_logits, oh_g), axis=-1)
    top_v, top_i = lax.top_k(pe, TOP_K)
    top_v = top_v / jnp.sum(top_v, axis=-1, keepdims=True)
    w_grp = jnp.sum(jax.nn.one_hot(top_i, EXPERTS_PER_GROUP, dtype=jnp.float32) * top_v[..., None], axis=1)
    combine = (p_sel[:, None, None] * oh_g[:, :, None] * w_grp[:, None, :]).reshape(N, N_EXPERTS).astype(h.dtype)
    a = jnp.einsum('nd,edh->neh', h, w1)
    b = jnp.einsum('nd,edh->neh', h, w3)
    act = jax.nn.silu(a) * b * combine[..., None]
    return jnp.einsum('neh,ehd->nd', act, w2)


def setup_inputs(seed: int = 0) -> dict:
    key = jax.random.key(seed)
    ks = iter(jax.random.split(key, 48))
    L, D = DEPTH, D_MODEL
    nrm = lambda shape, s: jax.random.normal(next(ks), shape, jnp.float32) * s
    uni = lambda shape, lo, hi: jax.random.uniform(next(ks), shape, jnp.float32, lo, hi)
    return {
        "x": nrm((BATCH, SEQ, D), 1.0),
        "c": nrm((BATCH, D), 1.0),
        "ctx": nrm((BATCH, CTX_LEN, D), 1.0),
        "c_ctx": nrm((D,), 1.0),
        "mod_w": nrm((L, D, 6 * D), 0.5 * D ** -0.5),
        "mod_b": nrm((L, 6 * D), 0.02),
        "norm1_g": 1.0 + nrm((L, D), 0.02),
        "norm2_g": 1.0 + nrm((L, D), 0.02),
        "w_in": nrm((L, D, D_IN), D ** -0.5),
        "w_out": nrm((L, D_MIX, D), D_MIX ** -0.5),
        "shift_prev": uni((L, RWKV_COLS), 0.0, 0.5),
        "shift_next": uni((L, RWKV_COLS), 0.0, 0.5),
        "decay_w0": nrm((L, 2, RWKV_W), 0.5),
        "decay_up": nrm((L, 2, DECAY_RANK, RWKV_W), DECAY_RANK ** -0.5),
        "iclr_a0": nrm((L, 2, RWKV_W), 0.5),
        "iclr_up": nrm((L, 2, ICLR_RANK, RWKV_W), 0.5 * ICLR_RANK ** -0.5),
        "gate_up": nrm((L, GATE_RANK, RWKV_W), GATE_RANK ** -0.5),
        "k_k": 0.85 + nrm((L, RWKV_W), 0.02),
        "k_a": 1.0 + nrm((L, RWKV_W), 0.02),
        "r_k": nrm((L, RWKV_W), 0.1),
        "gn_g": 1.0 + nrm((L, RWKV_W), 0.02),
        "gn_b": nrm((L, RWKV_W), 0.02),
        "q_norm_g": 1.0 + nrm((L, GQA_HEAD), 0.02),
        "k_norm_g": 1.0 + nrm((L, GQA_HEAD), 0.02),
        "mla_q_norm_g": 1.0 + nrm((L, MLA_Q_RANK), 0.02),
        "mla_w_uq": nrm((L, MLA_Q_RANK, MLA_HEADS * (MLA_NOPE + MLA_ROPE)), MLA_Q_RANK ** -0.5),
        "mla_kv_norm_g": 1.0 + nrm((L, MLA_KV_RANK), 0.02),
        "mla_w_ukv": nrm((L, MLA_KV_RANK, MLA_HEADS * (MLA_NOPE + MLA_V)), MLA_KV_RANK ** -0.5),
        "router_gw": nrm((L, D, N_GROUPS), D ** -0.5),
        "router_gb": nrm((L, N_GROUPS), 0.01),
        "router_ew": nrm((L, D, N_EXPERTS), D ** -0.5),
        "router_eb": nrm((L, N_EXPERTS), 0.01),
        "exp_w1": nrm((L, N_EXPERTS, D, D_EXPERT), D ** -0.5),
        "exp_w3": nrm((L, N_EXPERTS, D, D_EXPERT), D ** -0.5),
        "exp_w2": nrm((L, N_EXPERTS, D_EXPERT, D), D_EXPERT ** -0.5),
        "final_norm_g": 1.0 + nrm((D,), 0.02),
    }


def reference(x, c, ctx, c_ctx, mod_w, mod_b, norm1_g, norm2_g, w_in, w_out, shift_prev, shift_next,
              decay_w0, decay_up, iclr_a0, iclr_up, gate_up, k_k, k_a, r_k, gn_g, gn_b, q_norm_g, k_norm_g,
              mla_q_norm_g, mla_w_uq, mla_kv_norm_g, mla_w_ukv, router_gw, router_gb, router_ew, router_eb,
              exp_w1, exp_w3, exp_w2, final_norm_g):
    B, S, D = x.shape
    C = ctx.shape[1]
    rows = S // GRID_W
    row = jnp.repeat(jnp.arange(rows, dtype=jnp.float32), GRID_W)
    col = jnp.tile(jnp.arange(GRID_W, dtype=jnp.float32), rows)
    sc = jax.nn.silu(c)
    scc = jax.nn.silu(c_ctx)
    for l in range(DEPTH):
        need_ctx = l < DEPTH - 1
        m_lat = jnp.split((sc @ mod_w[l] + mod_b[l])[:, None, :], 6, axis=-1)
        m_ctx = jnp.split(scc @ mod_w[l] + mod_b[l], 6, axis=-1)
        h_lat = modulate(rmsnorm(x, norm1_g[l]), m_lat[0], m_lat[1])
        h_ctx = modulate(rmsnorm(ctx, norm1_g[l]), m_ctx[0], m_ctx[1])
        pr_l, pg_l, pm_l = split_cols(h_lat @ w_in[l], (RWKV_COLS, GQA_COLS, MLA_COLS))
        pr_c, pg_c, pm_c = split_cols(h_ctx @ w_in[l], (RWKV_COLS, GQA_COLS, MLA_COLS))
        o_r_l, o_r_c = rwkv_mixer(pr_l, pr_c, shift_prev[l], shift_next[l], decay_w0[l], decay_up[l],
                                  iclr_a0[l], iclr_up[l], gate_up[l], k_k[l], k_a[l], r_k[l], gn_g[l], gn_b[l], need_ctx)
        o_g_l, o_g_c = gqa_mixer(pg_l, pg_c, row, col, q_norm_g[l], k_norm_g[l], need_ctx)
        o_m_l, o_m_c = mla_mixer(pm_l, pm_c, row, col, mla_q_norm_g[l], mla_w_uq[l], mla_kv_norm_g[l], mla_w_ukv[l], need_ctx)
        x = x + m_lat[2] * (jnp.concatenate([o_r_l, o_g_l, o_m_l], axis=-1) @ w_out[l])
        h2 = modulate(rmsnorm(x, norm2_g[l]), m_lat[3], m_lat[4]).reshape(B * S, D)
        x = x + m_lat[5] * hier_moe(h2, router_gw[l], router_gb[l], router_ew[l], router_eb[l],
                                    exp_w1[l], exp_w3[l], exp_w2[l]).reshape(B, S, D)
        if need_ctx:
            ctx = ctx + m_ctx[2] * (jnp.concatenate([o_r_c, o_g_c, o_m_c], axis=-1) @ w_out[l])
            h2c = modulate(rmsnorm(ctx, norm2_g[l]), m_ctx[3], m_ctx[4]).reshape(B * C, D)
            ctx = ctx + m_ctx[5] * hier_moe(h2c, router_gw[l], router_gb[l], router_ew[l], router_eb[l],
                                            exp_w1[l], exp_w3[l], exp_w2[l]).reshape(B, C, D)
    return rmsnorm(x, final_norm_g)
```

```python
import numpy as np
from contextlib import ExitStack
import concourse.bass as bass
import concourse.mybir as mybir
from concourse.bass_utils import run_bass_kernel_spmd

F32 = mybir.dt.float32
BF16 = mybir.dt.bfloat16
AF = mybir.ActivationFunctionType
ALU = mybir.AluOpType
AX = mybir.AxisListType


class V:
    __slots__ = ("ap", "keys")

    def __init__(self, ap, keys):
        self.ap = ap
        self.keys = keys


class Tl:
    def __init__(self, t, name, psum=False):
        self.t = t
        self.name = name
        self.bk = (("BANK", name),) if psum else ()

    def __getitem__(self, idx):
        return V(self.t[idx], (self.name,) + self.bk)

    def k(self, sub, idx):
        return V(self.t[idx], ((self.name, sub),) + self.bk)

    def ks(self, subs, idx):
        return V(self.t[idx], tuple((self.name, s) for s in subs) + self.bk)


class Ev:
    __slots__ = ("prod", "count", "clock", "eng")

    def __init__(self, prod, count, clock, eng):
        self.prod = prod
        self.count = count
        self.clock = clock
        self.eng = eng


class Sched:
    NDMA = 48

    def __init__(self, nc, stack):
        self.nc = nc
        self.stack = stack
        self.E = {"pe": nc.tensor, "dve": nc.vector, "act": nc.scalar, "pool": nc.gpsimd, "sp": nc.sync}
        self.NR = 8
        self.sem = {e: [stack.enter_context(nc.semaphore("s_%s%d" % (e, i))) for i in range(self.NR)] for e in self.E if e != "sp"}
        self.cnt = {e: 0 for e in self.E}
        self.clock = {e: {} for e in self.E}
        self.dsem = [stack.enter_context(nc.semaphore("d%d" % i)) for i in range(self.NDMA)]
        self.dcnt = [0] * self.NDMA
        self.dnext = 0
        self.last_w = {}
        self.readers = {}
        self.nt = 0
        self.out_evs = []
        self.ninstr = 0
        self.prefix = ""
        self.bg = None
        self.in_bg = False
        self.tickc = 0
        self.bgk = 6

    def sb(self, shape, dt=F32, name=None, stack=None):
        self.nt += 1
        name = self.prefix + (name or ("t%d" % self.nt))
        t = (stack or self.stack).enter_context(self.nc.sbuf_tensor(name, list(shape), dt))
        return Tl(t, name)

    def ps(self, shape, dt=F32, name=None, stack=None):
        self.nt += 1
        name = self.prefix + (name or ("p%d" % self.nt))
        esz = 2 if dt == BF16 else 4
        t = (stack or self.stack).enter_context(self.nc.psum_tensor(name, [128, 2048 // esz], dt))
        n = 1
        for d_ in shape[1:]:
            n *= d_
        ap = t[0:shape[0], 0:n]
        if len(shape) == 3:
            ap = ap.rearrange("p (a b) -> p a b", a=shape[1])
        return Tl(ap, name, psum=True)

    def _semof(self, prod, count):
        if isinstance(prod, str):
            return self.sem[prod][(count - 1) % self.NR], (count - 1) // self.NR + 1
        return self.dsem[prod[1]], count

    def _wait(self, eng, ev):
        ck = self.clock[eng]
        if ck.get(ev.prod, 0) >= ev.count:
            return
        sm, cv = self._semof(ev.prod, ev.count)
        self.E[eng].wait_ge(sm, cv)
        self.ninstr += 1
        for p, c in ev.clock.items():
            if ck.get(p, 0) < c:
                ck[p] = c
        if ck.get(ev.prod, 0) < ev.count:
            ck[ev.prod] = ev.count

    def _deps(self, eng, reads, writes, is_dma):
        for v in reads:
            for k in v.keys:
                if isinstance(k, tuple) and k[0] == "BANK":
                    continue
                w = self.last_w.get(k)
                if w is not None:
                    if (not is_dma) and w.eng == eng and w.prod == eng and eng == "pe":
                        continue
                    self._wait(eng, w)
        for v in writes:
            for k in v.keys:
                w = self.last_w.get(k)
                if w is not None:
                    if not (w.prod == eng and not is_dma):
                        self._wait(eng, w)
                for r in self.readers.get(k, ()):
                    if r.prod == eng and not is_dma:
                        continue
                    self._wait(eng, r)

    def _record(self, ev, reads, writes):
        for v in reads:
            for k in v.keys:
                if isinstance(k, tuple) and k[0] == "BANK":
                    continue
                self.readers.setdefault(k, []).append(ev)
        for v in writes:
            for k in v.keys:
                self.last_w[k] = ev
                self.readers[k] = []

    def op(self, eng, fn, reads, writes):
        bks = set()
        for v in list(reads) + list(writes):
            for k in v.keys:
                if isinstance(k, tuple) and k[0] == "BANK":
                    bks.add(k)
        if bks:
            writes = list(writes) + [V(None, tuple(bks))]
        self._deps(eng, reads, writes, False)
        ins = fn(self.E[eng])
        self.cnt[eng] += 1
        c = self.cnt[eng]
        ins.then_inc(self.sem[eng][(c - 1) % self.NR], 1)
        self.ninstr += 1
        ck = self.clock[eng]
        ev = Ev(eng, c, dict(ck), eng)
        ev.clock[eng] = c
        self._record(ev, reads, writes)
        self._tick()
        return ev

    def _tick(self):
        if self.bg is not None and not self.in_bg:
            self.tickc += 1
            if self.tickc % self.bgk == 0:
                self.in_bg = True
                try:
                    next(self.bg)
                except StopIteration:
                    self.bg = None
                self.in_bg = False

    def drain_bg(self):
        if self.bg is not None:
            self.in_bg = True
            for _ in self.bg:
                pass
            self.bg = None
            self.in_bg = False

    def dma(self, q, out, in_, is_output=False, **kw):
        self._deps(q, [in_], [out], True)
        s = self.dnext
        self.dnext = (self.dnext + 1) % self.NDMA
        prod = ("dma", s)
        prev = self.dcnt[s]
        if prev and self.clock[q].get(prod, 0) < prev:
            self.E[q].wait_ge(self.dsem[s], prev)
            self.clock[q][prod] = prev
        ins = self.E[q].dma_start(out=out.ap, in_=in_.ap, **kw)
        self.dcnt[s] += 16
        ins.then_inc(self.dsem[s], 16)
        self.ninstr += 1
        ev = Ev(prod, self.dcnt[s], dict(self.clock[q]), q)
        self._record(ev, [in_], [out])
        if is_output:
            self.out_evs.append(ev)
        return ev

    def coll(self, kind, ins, outs, groups):
        q = "pool"
        for o in outs:
            self._deps(q, ins, [o], True)
        s = self.dnext
        self.dnext = (self.dnext + 1) % self.NDMA
        prod = ("dma", s)
        prev = self.dcnt[s]
        if prev and self.clock[q].get(prod, 0) < prev:
            self.E[q].wait_ge(self.dsem[s], prev)
            self.clock[q][prod] = prev
        ins_ = self.E[q].collective_compute(kind, ALU.bypass, groups, [v.ap for v in ins], [v.ap for v in outs])
        self.dcnt[s] += 16
        ins_.then_inc(self.dsem[s], 16)
        self.ninstr += 1
        ev = Ev(prod, self.dcnt[s], dict(self.clock[q]), q)
        self._record(ev, ins, outs)
        return ev

    def finish(self):
        for ev in self.out_evs:
            self._wait("sp", ev)
        for e in self.E:
            if e != "sp" and self.cnt[e]:
                self._wait("sp", Ev(e, self.cnt[e], {}, e))

    def mm(self, out, lhsT, rhs, start=True, stop=True):
        return self.op("pe", lambda e: e.matmul(out.ap, lhsT.ap, rhs.ap, start=start, stop=stop), [lhsT, rhs], [out])

    def tr(self, out, in_, ident):
        return self.op("pe", lambda e: e.transpose(out.ap, in_.ap, ident.ap), [in_, ident], [out])

    def act(self, out, in_, func, bias=None, scale=None, accum=None, eng="act"):
        kw = {}
        rd = [in_]
        wr = [out]
        if bias is not None:
            if isinstance(bias, V):
                kw["bias"] = bias.ap
                rd.append(bias)
            else:
                kw["bias"] = bias
        if scale is not None:
            if isinstance(scale, V):
                kw["scale"] = scale.ap
                rd.append(scale)
            else:
                kw["scale"] = scale
        if accum is not None:
            kw["accum_out"] = accum.ap
            wr.append(accum)
        return self.op("act", lambda e: e.activation(out.ap, in_.ap, func, **kw), rd, wr)

    def tt(self, eng, out, a, b, op):
        return self.op(eng, lambda e: e.tensor_tensor(out.ap, a.ap, b.ap, op), [a, b], [out])

    def ts(self, eng, out, a, s1, s2, op0, op1=None, accum=None):
        rd = [a]
        wr = [out]
        a1 = s1
        a2 = s2
        if isinstance(s1, V):
            rd.append(s1)
            a1 = s1.ap
        if isinstance(s2, V):
            rd.append(s2)
            a2 = s2.ap
        kw = {}
        if op1 is not None:
            kw["op1"] = op1
        if accum is not None:
            kw["accum_out"] = accum.ap
            wr.append(accum)
        return self.op(eng, lambda e: e.tensor_scalar(out.ap, a.ap, a1, a2, op0, **kw), rd, wr)

    def stt(self, eng, out, a, s, b, op0, op1):
        rd = [a, b]
        sa = s
        if isinstance(s, V):
            rd.append(s)
            sa = s.ap
        return self.op(eng, lambda e: e.scalar_tensor_tensor(out.ap, a.ap, sa, b.ap, op0, op1), rd, [out])

    def copy(self, eng, out, in_):
        if eng == "act":
            return self.op("act", lambda e: e.copy(out.ap, in_.ap), [in_], [out])
        return self.op(eng, lambda e: e.tensor_copy(out.ap, in_.ap), [in_], [out])

    def memset(self, eng, out, val):
        return self.op(eng, lambda e: e.memset(out.ap, val), [], [out])

    def reduce(self, eng, out, in_, op, axis=AX.X):
        return self.op(eng, lambda e: e.tensor_reduce(out.ap, in_.ap, axis, op), [in_], [out])

    def recip(self, out, in_):
        return self.op("dve", lambda e: e.reciprocal(out.ap, in_.ap), [in_], [out])


def dram_in(nc, name, shape, dt=F32):
    return V(nc.dram_tensor(name, list(shape), dt, kind="ExternalInput").ap(), ("dram_" + name,))


def dram_out(nc, name, shape, dt=F32):
    return V(nc.dram_tensor(name, list(shape), dt, kind="ExternalOutput").ap(), ("dram_" + name,))


def dv(v, idx_fn):
    return V(idx_fn(v.ap), v.keys)


D = 2048
DIN = 4032
NT = 9
TOK = NT * 128
NCORES = 8
CTX = {}
CW = 504


def barrier(S):
    engs = ["pe", "dve", "act", "pool"]
    evs = [Ev(e, S.cnt[e], {}, e) for e in engs if S.cnt[e]]
    for e in engs + ["sp"]:
        for ev in evs:
            if ev.prod != e:
                S._wait(e, ev)
    for s in range(S.NDMA):
        if S.dcnt[s]:
            ev = Ev(("dma", s), S.dcnt[s], {}, "sp")
            for e in engs + ["sp"]:
                S._wait(e, ev)


def rr(i, engs=("dve", "act", "pool")):
    return engs[i % len(engs)]


def build_A():
    nc = CTX["nc"]
    io = CTX["io"]
    xin = io["xin"]
    cT = io["cT"]
    mod_w = io["mod_w"]
    mod_b = io["mod_b"]
    g1 = io["g1"]
    w_in = io["w_in"]
    ident = io["ident"]
    P = io["P"]
    mod = io["mod"]
    with ExitStack() as st:
        S = CTX["S"]
        S.stack = st
        idt = S.sb([128, 128], name="idt")
        S.dma("sp", idt[:], ident)
        idb = S.sb([128, 128], BF16, name="idb")
        S.copy("dve", idb[:], idt[:])
        with ExitStack() as st1:
          if CTX.get("do_mod", True):
              scT = S.sb([128, 32], name="scT", stack=st1)
              craw = S.sb([128, 32], name="craw", stack=st1)
              S.dma("sp", craw[:], cT)
              S.act(scT[:], craw[:], AF.Silu)
              mb = S.sb([2, 6 * D], name="mb", stack=st1)
              S.dma("act", mb[0:1, :], mod_b)
              S.dma("act", mb[1:2, :], mod_b)
              NB = 3
              wbuf = [S.sb([128, 16, 512], name="modw%d" % i, stack=st1) for i in range(NB)]
              pm = [S.ps([2, 512], name="pm%d" % i, stack=st1) for i in range(2)]
              mo = [S.sb([2, 512], name="mo%d" % i, stack=st1) for i in range(2)]
              for j in range(24):
                  wb = wbuf[j % NB]
                  src = dv(mod_w, lambda a: a[:, j * 512:(j + 1) * 512].rearrange("(c p) n -> p c n", p=128))
                  for h in range(2):
                      S.dma(("sp", "pool")[h], wb.k(h, (slice(None), slice(h * 8, h * 8 + 8), slice(None))),
                            dv(src, lambda a: a[:, h * 8:h * 8 + 8, :]))
                  for c in range(16):
                      S.mm(pm[j % 2][:], scT[:, 2 * c:2 * c + 2], wb.k(c // 8, (slice(None), c, slice(None))),
                           start=(c == 0), stop=(c == 15))
                  S.tt("dve", mo[j % 2][:], pm[j % 2][:], mb[:, j * 512:(j + 1) * 512], ALU.add)
                  S.dma("act", dv(mod, lambda a: a[:, j * 512:(j + 1) * 512]), mo[j % 2][:], is_output=True)
              barrier(S)
        g1b = S.sb([128, D], name="g1b")
        S.dma("sp", g1b[:], dv(g1, lambda a: a.partition_broadcast(128)))
        gs = []
        sh = []
        for r in range(2):
            sc_b = S.sb([128, D], name="scb%d" % r)
            sh_b = S.sb([128, D], name="shb%d" % r)
            S.dma("sp", sh_b[:], dv(mod, lambda a: a[r:r + 1, 0:D].partition_broadcast(128)))
            S.dma("pool", sc_b[:], dv(mod, lambda a: a[r:r + 1, D:2 * D].partition_broadcast(128)))
            S.stt("dve", sc_b[:], sc_b[:], 1.0, g1b[:], ALU.add, ALU.mult)
            gs.append(sc_b)
            sh.append(sh_b)
        hT = S.sb([128, NT, 16, 128], BF16, name="hT")
        xt = [S.sb([128, D], name="xt%d" % i) for i in range(2)]
        tmp = S.sb([128, D], name="tmpA")
        hb = S.sb([128, D], BF16, name="hb")
        stat = S.sb([128, NT, 4], name="statA")
        ptr = [S.ps([128, 4, 128], BF16, name="ptr%d" % i) for i in range(2)]
        for i in range(NT):
            r = 1 if i == 0 else 0
            x_t = xt[i % 2]
            S.dma(("sp", "pool")[i % 2], x_t[:], dv(xin, lambda a: a[i * 128:(i + 1) * 128, :]))
            S.act(tmp[:], x_t[:], AF.Square, accum=stat[:, i, 0:1])
            S.ts("dve", stat[:, i, 1:2], stat[:, i, 0:1], 1.0 / D, 1e-6, ALU.mult, ALU.add)
            S.act(stat[:, i, 2:3], stat[:, i, 1:2], AF.Sqrt)
            S.recip(stat[:, i, 3:4], stat[:, i, 2:3])
            S.stt("dve", tmp[:], x_t[:], stat[:, i, 3:4], gs[r][:], ALU.mult, ALU.mult)
            S.tt("pool", hb[:], tmp[:], sh[r][:], ALU.add)
            for q in range(4):
                pt = ptr[q % 2]
                for c4 in range(4):
                    c = q * 4 + c4
                    S.tr(pt[:, c4, :], hb[:, c * 128:(c + 1) * 128], idb[:])
                S.copy(("dve", "act")[q % 2], hT[:, i, q * 4:q * 4 + 4, :], pt[:])
        NS = 4
        wst = [S.sb([128, 4, CW], name="wst%d" % i) for i in range(NS)]
        wbf = [S.sb([128, 16, CW], BF16, name="wbf%d" % i) for i in range(2)]
        po = [S.ps([128, CW], name="poA%d" % i) for i in range(3)]
        ot = [S.sb([128, CW], name="otA%d" % i) for i in range(3)]
        n = 0
        k = 0
        for j in range(DIN // CW):
            wb = wbf[j % 2]
            for q in range(4):
                ws = wst[k % NS]
                S.dma(("sp", "pool")[k % 2], ws[:],
                      dv(w_in, lambda a: a[q * 512:(q + 1) * 512, j * CW:(j + 1) * CW].rearrange("(c p) n -> p c n", p=128)))
                S.copy(rr(k), wb.k(q, (slice(None), slice(q * 4, q * 4 + 4), slice(None))), ws[:])
                k += 1
            for i in range(NT):
                p_ = po[n % 3]
                o_ = ot[n % 3]
                for c in range(16):
                    S.mm(p_[:], hT[:, i, c, :], wb.k(c // 4, (slice(None), c, slice(None))), start=(c == 0), stop=(c == 15))
                S.copy(("dve", "act")[n % 2], o_[:], p_[:])
                S.dma("act" if n % 2 else "sp", dv(P, lambda a: a[i * 128:(i + 1) * 128, j * CW:(j + 1) * CW]), o_[:], is_output=True)
                n += 1
        barrier(S)
        print("A ninstr", S.ninstr)
    return nc


def core_tokens(x, ctx, core):
    b, h = core // 2, core % 2
    return np.concatenate([ctx[b, h * 128:(h + 1) * 128], x[b, h * 1024:(h + 1) * 1024]], axis=0)


def cT_layout(c_b, c_ctx):
    cv = np.stack([c_b, c_ctx], axis=0)
    return np.ascontiguousarray(cv.reshape(2, 16, 128).transpose(2, 1, 0).reshape(128, 32))


_NC = {}


def get_nc(name, fn):
    if name not in _NC:
        _NC[name] = fn()
    return _NC[name]


def run_A(x, ctx, c, c_ctx, mod_w_l, mod_b_l, g1_l, w_in_l):
    nc = get_nc("A", build_A)
    ident = np.eye(128, dtype=np.float32)
    maps = []
    for core in range(NCORES):
        maps.append(dict(xin=np.ascontiguousarray(core_tokens(x, ctx, core)), cT=cT_layout(c[core // 2], c_ctx),
                         mod_w=mod_w_l, mod_b=mod_b_l.reshape(1, -1), g1=g1_l.reshape(1, -1), w_in=w_in_l, ident=ident))
    res = run_bass_kernel_spmd(nc, maps, core_ids=list(range(NCORES)))
    return [r["P"] for r in res.results], [r["mod"] for r in res.results]


NTB = 18
TB = NTB * 128
PA_W = 1472


def rstd_of(S, out, ss, width, eps, tmp):
    S.ts("dve", tmp[0], ss, 1.0 / width, eps, ALU.mult, ALU.add)
    S.act(tmp[1], tmp[0], AF.Sqrt)
    S.recip(out, tmp[1])


def rope_tm(S, eng, out, x, cos, sin, nh, qd, t1, t2):
    def v5(v):
        return v.ap.rearrange("p (h a two j) -> p h a two j", h=nh, a=2, two=2)
    xv = v5(x)
    ov = v5(out)
    x1 = V(xv[:, :, :, 0, :], x.keys)
    x2 = V(xv[:, :, :, 1, :], x.keys)
    o1 = V(ov[:, :, :, 0, :], out.keys)
    o2 = V(ov[:, :, :, 1, :], out.keys)
    def bc(v):
        a = v.ap.rearrange("p (a j) -> p a j", a=2).unsqueeze(1).to_broadcast([128, nh, 2, qd])
        return V(a, v.keys)
    c = bc(cos)
    s = bc(sin)
    def sh(v):
        return V(v.ap.rearrange("p (h a j) -> p h a j", h=nh, a=2), v.keys)
    a1 = sh(t1)
    a2 = sh(t2)
    S.tt(eng, a1, x1, c, ALU.mult)
    S.tt(eng, a2, x2, s, ALU.mult)
    S.tt(eng, o1, a1, a2, ALU.subtract)
    S.tt(eng, a1, x1, s, ALU.mult)
    S.tt(eng, a2, x2, c, ALU.mult)
    S.tt(eng, o2, a1, a2, ALU.add)


def build_Battn():
    nc = CTX["nc"]
    io = CTX["io"]
    Pa = io["Pa"]
    gq = io["gq"]
    gk = io["gk"]
    gmq = io["gmq"]
    gmkv = io["gmkv"]
    wuq = io["wuq"]
    wukv = io["wukv"]
    cosG = io["cosG"]
    sinG = io["sinG"]
    cosM = io["cosM"]
    sinM = io["sinM"]
    ident = io["ident"]
    Oa = io["Oa"]
    SC_G = 128 ** -0.5
    SC_M = 192 ** -0.5
    with ExitStack() as st:
        S = CTX["S"]
        S.stack = st
        idt = S.sb([128, 128], name="idt")
        S.dma("sp", idt[:], ident)
        idb = S.sb([128, 128], BF16, name="idb")
        S.copy("dve", idb[:], idt[:])
        def bload(src, n, name, q="sp"):
            t = S.sb([128, n], name=name)
            S.dma(q, t[:], dv(src, lambda a: a.partition_broadcast(128)))
            return t
        gq_b = bload(gq, 128, "gq_b")
        gk_b = bload(gk, 128, "gk_b", "act")
        gmq_b = bload(gmq, 384, "gmq_b", "pool")
        gmkv_b = bload(gmkv, 256, "gmkv_b")
        wuq_f = S.sb([128, 3, 384], name="wuq_f")
        S.dma("sp", wuq_f[:], dv(wuq, lambda a: a.rearrange("(c p) n -> p c n", p=128)))
        wuq_b = S.sb([128, 3, 384], BF16, name="wuq_b")
        S.copy("pool", wuq_b[:], wuq_f[:])
        wukv_f = S.sb([128, 2, 512], name="wukv_f")
        S.dma("act", wukv_f[:], dv(wukv, lambda a: a.rearrange("(c p) n -> p c n", p=128)))
        wukv_b = S.sb([128, 2, 512], BF16, name="wukv_b")
        S.copy("pool", wukv_b[:], wukv_f[:])
        KT = S.sb([128, TB], BF16, name="KT")
        Vg = S.sb([128, NTB, 129], BF16, name="Vg")
        KnT = S.sb([128, 2, TB], BF16, name="KnT")
        krT = S.sb([64, TB], BF16, name="krT")
        Vm = S.sb([128, NTB, 2, 129], BF16, name="Vm")
        S.memset("pool", Vg[:, :, 128:129], 1.0)
        S.memset("pool", Vm[:, :, :, 128:129], 1.0)
        kvt = [S.sb([128, 576], name="kvt%d" % i) for i in range(2)]
        cs = [S.sb([128, 192], name="cs%d" % i) for i in range(2)]
        junk = S.sb([128, 512], name="junk")
        stat = [S.sb([128, 16], name="stat%d" % i) for i in range(2)]
        f1 = S.sb([128, 512], name="f1")
        f2 = S.sb([128, 512], name="f2")
        r1 = S.sb([128, 256], name="r1")
        r2 = S.sb([128, 256], name="r2")
        b1 = S.sb([128, 512], BF16, name="b1")
        b2 = S.sb([128, 384], BF16, name="b2")
        b3 = S.sb([128, 128], BF16, name="b3")
        ckT = S.sb([128, 2, 128], BF16, name="ckT")
        ptr = [S.ps([128, 4, 128], BF16, name="ptr%d" % i) for i in range(1)]
        pmm = [S.ps([128, 512], name="pmm%d" % i) for i in range(1)]
        npm = [0]
        def next_pmm():
            npm[0] += 1
            return pmm[npm[0] % 1]

        def load_cs(i, buf):
            p0 = (i - 2) * 128
            S.dma("sp", buf[:, 0:64], dv(cosG, lambda a: a[p0:p0 + 128, :]))
            S.dma("act", buf[:, 64:128], dv(sinG, lambda a: a[p0:p0 + 128, :]))
            S.dma("sp", buf[:, 128:160], dv(cosM, lambda a: a[p0:p0 + 128, :]))
            S.dma("act", buf[:, 160:192], dv(sinM, lambda a: a[p0:p0 + 128, :]))

        for i in range(NTB):
            lat = i >= 2
            kv = kvt[i % 2]
            st_ = stat[i % 2]
            csb = cs[i % 2]
            r0 = i * 128
            S.dma("sp", kv[:, 0:256], dv(Pa, lambda a: a[r0:r0 + 128, 512:768]))
            S.dma("pool", kv[:, 256:576], dv(Pa, lambda a: a[r0:r0 + 128, 1152:1472]))
            if lat:
                load_cs(i, csb)
            S.act(junk[:, 0:128], kv[:, 0:128], AF.Square, accum=st_[:, 0:1])
            rstd_of(S, st_[:, 1:2], st_[:, 0:1], 128, 1e-6, (st_[:, 2:3], st_[:, 3:4]))
            S.stt("dve", f1[:, 0:128], kv[:, 0:128], st_[:, 1:2], gk_b[:], ALU.mult, ALU.mult)
            if lat:
                rope_tm(S, "pool", b3[:], f1[:, 0:128], csb[:, 0:64], csb[:, 64:128], 1, 32, r1[:, 0:64], r2[:, 0:64])
            else:
                S.copy("pool", b3[:], f1[:, 0:128])
            S.tr(ptr[0][:, 0, :], b3[:], idb[:])
            S.copy("dve", KT[:, r0:r0 + 128], ptr[0][:, 0, :])
            S.copy("pool", Vg[:, i, 0:128], kv[:, 128:256])
            S.act(junk[:, 0:256], kv[:, 256:512], AF.Square, accum=st_[:, 4:5])
            rstd_of(S, st_[:, 5:6], st_[:, 4:5], 256, 1e-6, (st_[:, 6:7], st_[:, 7:8]))
            S.stt("dve", b1[:, 0:256], kv[:, 256:512], st_[:, 5:6], gmkv_b[:], ALU.mult, ALU.mult)
            for c in range(2):
                S.tr(ptr[0][:, 1 + c, :], b1[:, c * 128:(c + 1) * 128], idb[:])
            S.copy("dve", ckT[:], ptr[0][:, 1:3, :])
            pk = next_pmm()
            for hh in range(2):
                for c in range(2):
                    S.mm(pk[:, hh * 128:(hh + 1) * 128], wukv_b[:, c, hh * 256:hh * 256 + 128], ckT[:, c, :],
                         start=(c == 0), stop=(c == 1))
            S.copy("act", KnT[:, :, r0:r0 + 128], vw(pk[:, 0:256], lambda a: a.rearrange("p (h t) -> p h t", h=2)))
            pv = next_pmm()
            for hh in range(2):
                for c in range(2):
                    S.mm(pv[:, hh * 128:(hh + 1) * 128], ckT[:, c, :], wukv_b[:, c, hh * 256 + 128:hh * 256 + 256],
                         start=(c == 0), stop=(c == 1))
            S.copy("dve", Vm[:, i, :, 0:128], vw(pv[:, 0:256], lambda a: a.rearrange("p (h d) -> p h d", h=2)))
            if lat:
                rope_tm(S, "pool", b2[:, 0:64], kv[:, 512:576], csb[:, 128:160], csb[:, 160:192], 1, 16, r1[:, 64:96], r2[:, 64:96])
            else:
                S.copy("pool", b2[:, 0:64], kv[:, 512:576])
            S.tr(ptr[0][0:64, 3, :], b2[:, 0:64], idb[:])
            S.copy("dve", krT[:, r0:r0 + 128], ptr[0][0:64, 3, :])

        QT = S.sb([128, 4, 512], BF16, name="QT")
        cqT = S.sb([128, 3, 512], BF16, name="cqT")
        QnT = S.sb([128, 2, 512], BF16, name="QnT")
        QrT = S.sb([64, 2, 512], BF16, name="QrT")
        qt = [S.sb([128, 896], name="qt%d" % i) for i in range(2)]
        Et = [S.sb([128, 512], BF16, name="Et%d" % i) for i in range(3)]
        psT = [S.ps([128, 512], name="psT%d" % i) for i in range(2)]
        pacc = [S.ps([128, 512], name="pacc%d" % i) for i in range(4)]
        otile = [S.sb([128, 768], name="otile%d" % i) for i in range(4)]
        rs = S.sb([128, 8], name="rs")
        nE = [0]
        nrs = [0]

        def attend(nq, ktiles, score_ops, vfn, scale, ocol):
            ntq = nq // 128
            nk = len(ktiles)
            def scores(kt):
                ps = psT[nE[0] % 2]
                E = Et[nE[0] % 3]
                nE[0] += 1
                ops = score_ops(kt)
                for oi, (l_, r_) in enumerate(ops):
                    S.mm(ps[:, 0:nq], l_, r_, start=(oi == 0), stop=(oi == len(ops) - 1))
                return ps, E
            cur = scores(ktiles[0])
            for ki, kt in enumerate(ktiles):
                ps, E = cur
                S.act(E[:, 0:nq], ps[:, 0:nq], AF.Exp, scale=scale)
                if ki + 1 < nk:
                    cur = scores(ktiles[ki + 1])
                vv = vfn(kt)
                for j in range(ntq):
                    S.mm(pacc[j][:, 0:129], E[:, j * 128:(j + 1) * 128], vv, start=(ki == 0), stop=(ki == nk - 1))
            for j in range(ntq):
                c = nrs[0] % 8
                nrs[0] += 1
                S.recip(rs[:, c:c + 1], pacc[j][:, 128:129])
                S.ts("dve", otile[j][:, ocol:ocol + 128], pacc[j][:, 0:128], rs[:, c:c + 1], None, ALU.mult)

        if CTX.get("own_only", False):
            groups = [([0], [0, 1])] + [(list(range(2 + 4 * g, 6 + 4 * g)), list(range(NTB))) for g in range(2)]
        else:
            groups = [(list(range(0, 2)), list(range(0, 2)))] + [(list(range(2 + 4 * g, 6 + 4 * g)), list(range(NTB))) for g in range(4)]
        nq_ = 0
        for (qtiles, ktiles) in groups:
            nq = len(qtiles) * 128
            for j, i in enumerate(qtiles):
                lat = i >= 2
                q_ = qt[nq_ % 2]
                st_ = stat[nq_ % 2]
                csb = cs[nq_ % 2]
                nq_ += 1
                r0 = i * 128
                S.dma("sp", q_[:, 0:512], dv(Pa, lambda a: a[r0:r0 + 128, 0:512]))
                S.dma("pool", q_[:, 512:896], dv(Pa, lambda a: a[r0:r0 + 128, 768:1152]))
                if lat:
                    load_cs(i, csb)
                S.tt("pool", f1[:], q_[:, 0:512], q_[:, 0:512], ALU.mult)
                S.reduce("dve", st_[:, 8:12], V(f1.t[:].rearrange("p (h d) -> p h d", h=4), (f1.name,)), ALU.add)
                S.ts("dve", st_[:, 12:16], st_[:, 8:12], 1.0 / 128, 1e-6, ALU.mult, ALU.add)
                S.act(st_[:, 8:12], st_[:, 12:16], AF.Sqrt)
                S.recip(st_[:, 12:16], st_[:, 8:12])
                S.tt("dve", V(f2.t[:].rearrange("p (h d) -> p h d", h=4), (f2.name,)),
                     V(q_.t[:, 0:512].rearrange("p (h d) -> p h d", h=4), (q_.name,)),
                     V(st_.t[:, 12:16].unsqueeze(2).to_broadcast([128, 4, 128]), (st_.name,)), ALU.mult)
                if lat:
                    S.tt("pool", V(f1.t[:].rearrange("p (h d) -> p h d", h=4), (f1.name,)),
                         V(f2.t[:].rearrange("p (h d) -> p h d", h=4), (f2.name,)),
                         V(gq_b.t[:].unsqueeze(1).to_broadcast([128, 4, 128]), (gq_b.name,)), ALU.mult)
                    rope_tm(S, "pool", b1[:], f1[:], csb[:, 0:64], csb[:, 64:128], 4, 32, r1[:], r2[:])
                else:
                    S.tt("pool", V(b1.t[:].rearrange("p (h d) -> p h d", h=4), (b1.name,)),
                         V(f2.t[:].rearrange("p (h d) -> p h d", h=4), (f2.name,)),
                         V(gq_b.t[:].unsqueeze(1).to_broadcast([128, 4, 128]), (gq_b.name,)), ALU.mult)
                for h in range(4):
                    S.tr(ptr[0][:, h, :], b1[:, h * 128:(h + 1) * 128], idb[:])
                S.copy("dve", QT[:, :, j * 128:(j + 1) * 128], ptr[0][:])
                S.act(junk[:, 0:384], q_[:, 512:896], AF.Square, accum=st_[:, 4:5])
                rstd_of(S, st_[:, 5:6], st_[:, 4:5], 384, 1e-6, (st_[:, 6:7], st_[:, 7:8]))
                S.stt("dve", b2[:], q_[:, 512:896], st_[:, 5:6], gmq_b[:], ALU.mult, ALU.mult)
                for c in range(3):
                    S.tr(ptr[0][:, c, :], b2[:, c * 128:(c + 1) * 128], idb[:])
                S.copy("dve", cqT[:, :, j * 128:(j + 1) * 128], ptr[0][:, 0:3, :])
                pq = next_pmm()
                for hh in range(2):
                    for c in range(3):
                        S.mm(pq[:, hh * 64:(hh + 1) * 64], cqT[:, c, j * 128:(j + 1) * 128],
                             wuq_b[:, c, hh * 192 + 128:hh * 192 + 192], start=(c == 0), stop=(c == 2))
                if lat:
                    S.copy("act", f2[:, 0:128], pq[:, 0:128])
                    rope_tm(S, "pool", b3[:], f2[:, 0:128], csb[:, 128:160], csb[:, 160:192], 2, 16, r1[:, 0:64], r2[:, 0:64])
                else:
                    S.copy("act", b3[:], pq[:, 0:128])
                for hh in range(2):
                    S.tr(ptr[0][0:64, hh, :], b3[:, hh * 64:(hh + 1) * 64], idb[:])
                S.copy("dve", QrT[:, :, j * 128:(j + 1) * 128], ptr[0][0:64, 0:2, :])
            for hh in range(2):
                pq = next_pmm()
                for c in range(3):
                    S.mm(pq[:, 0:nq], wuq_b[:, c, hh * 192:hh * 192 + 128], cqT[:, c, 0:nq], start=(c == 0), stop=(c == 2))
                S.copy("act", QnT[:, hh, 0:nq], pq[:, 0:nq])
            for h in range(4):
                attend(nq, ktiles, lambda kt: [(KT[:, kt * 128:(kt + 1) * 128], QT[:, h, 0:nq])],
                       lambda kt: Vg[:, kt, :], SC_G, h * 128)
            for hh in range(2):
                attend(nq, ktiles, lambda kt: [(KnT[:, hh, kt * 128:(kt + 1) * 128], QnT[:, hh, 0:nq]),
                                               (krT[:, kt * 128:(kt + 1) * 128], QrT[:, hh, 0:nq])],
                       lambda kt: Vm[:, kt, hh, :], SC_M, 512 + hh * 128)
            for j, i in enumerate(qtiles):
                S.dma(("sp", "act")[j % 2], dv(Oa, lambda a: a[i * 128:(i + 1) * 128, :]), otile[j][:], is_output=True)
        barrier(S)
        print("Battn ninstr", S.ninstr)
    return nc


def rope_tables():
    t = np.arange(2048)
    row = (t // 64).astype(np.float32)
    col = (t % 64).astype(np.float32)
    def tab(q):
        inv = (10000.0 ** (-np.arange(q, dtype=np.float32) / q)).astype(np.float32)
        ar = row[:, None] * inv[None, :]
        ac = col[:, None] * inv[None, :]
        return (np.concatenate([np.cos(ar), np.cos(ac)], 1).astype(np.float32),
                np.concatenate([np.sin(ar), np.sin(ac)], 1).astype(np.float32))
    cG, sG = tab(32)
    cM, sM = tab(16)
    return cG, sG, cM, sM


def attn_cols(g):
    q0 = 1792
    cols = list(range(q0 + 512 * g, q0 + 512 * g + 512))
    cols += list(range(q0 + 1024 + 128 * g, q0 + 1024 + 128 * g + 128))
    cols += list(range(q0 + 1280 + 128 * g, q0 + 1280 + 128 * g + 128))
    m0 = 1792 + 1536
    cols += list(range(m0, m0 + 384 + 256 + 64))
    return np.array(cols)


def run_Battn(Pfull, q_norm_g, k_norm_g, mla_q_norm_g, mla_w_uq, mla_kv_norm_g, mla_w_ukv):
    nc = get_nc("Battn", build_Battn)
    ident = np.eye(128, dtype=np.float32)
    cG, sG, cM, sM = rope_tables()
    maps = []
    for core in range(NCORES):
        b, g = core // 2, core % 2
        wuq = np.concatenate([mla_w_uq[:, (2 * g + hh) * 192:(2 * g + hh + 1) * 192] for hh in range(2)], 1)
        wukv = np.concatenate([mla_w_ukv[:, (2 * g + hh) * 256:(2 * g + hh + 1) * 256] for hh in range(2)], 1)
        maps.append(dict(Pa=np.ascontiguousarray(Pfull[b][:, attn_cols(g)]), gq=q_norm_g.reshape(1, -1), gk=k_norm_g.reshape(1, -1),
                         gmq=mla_q_norm_g.reshape(1, -1), gmkv=mla_kv_norm_g.reshape(1, -1),
                         wuq=np.ascontiguousarray(wuq), wukv=np.ascontiguousarray(wukv),
                         cosG=cG, sinG=sG, cosM=cM, sinM=sM, ident=ident))
    res = run_bass_kernel_spmd(nc, maps, core_ids=list(range(NCORES)))
    return [r["Oa"] for r in res.results]


DECAY_SCALE = float(np.exp(-0.5))
GN_EPS = 64e-5


def vw(v, fn):
    return V(fn(v.ap), v.keys)


def h4(v, n=64):
    return vw(v, lambda a: a.rearrange("p (h d) -> p h d", h=4))


DBG = {"units": 2 * NTB, "stage": 99}


def build_Brwkv():
    nc = CTX["nc"]
    io = CTX["io"]
    Pr = io["Pr"]
    mup = io["mup"]
    mun = io["mun"]
    w0a0 = io["w0a0"]
    wup = io["wup"]
    aup = io["aup"]
    gup = io["gup"]
    vecs = io["vecs"]
    consts = io["consts"]
    Or = io["Or"]
    with ExitStack() as st:
        S = CTX["S"]
        S.stack = st
        cst = S.sb([128, 6, 128], name="cst")
        S.dma("sp", cst[:], dv(consts, lambda a: a.rearrange("p (c n) -> p c n", c=6)))
        ident = cst[:, 5, :]
        same = cst[:, 4, :]
        blockind = vw(cst[:, 4, :], lambda a: a[:, 0:128:64])
        def bload(src, n, name, q="sp"):
            t = S.sb([128, n], name=name)
            S.dma(q, t[:], dv(src, lambda a: a.partition_broadcast(128)))
            return t
        mup_b = bload(mup, 1024, "mup_b")
        mun_b = bload(mun, 1024, "mun_b", "act")
        w0a0_b = bload(w0a0, 1024, "w0a0_b", "pool")
        vec_b = bload(vecs, 1280, "vec_b")
        kk_b = vec_b[:, 0:256]
        ka_b = vec_b[:, 256:512]
        rk_b = vec_b[:, 512:768]
        gng_b = vec_b[:, 768:1024]
        gnb_b = vec_b[:, 1024:1280]
        oka_b = S.sb([128, 256], name="oka_b")
        S.ts("dve", oka_b[:], ka_b, -1.0, 1.0, ALU.mult, ALU.add)
        wup_s = S.sb([64, 512], name="wup_s")
        aup_s = S.sb([64, 512], name="aup_s")
        gup_s = S.sb([128, 256], name="gup_s")
        S.dma("sp", wup_s[:], wup)
        S.dma("act", aup_s[:], aup)
        S.dma("pool", gup_s[:], gup)
        Yd = [S.sb([128, NTB, 256], name="Yd%d" % d) for d in range(2)]
        vS = S.sb([128, NTB, 256], name="vS")
        gS = S.sb([128, NTB, 256], name="gS")
        bon = S.sb([128, NTB, 8], name="bon")
        A = [S.sb([64, 4, 64], name="A%d" % d) for d in range(2)]
        for d in range(2):
            S.memset("pool", A[d][:], 0.0)
        pt = S.sb([128, 1024], name="pt")
        prv = S.sb([128, 1024], name="prv")
        nx = S.sb([128, 1024], name="nx")
        zt = S.sb([128, 1024], name="zt")
        def T256(name):
            return S.sb([128, 256], name=name)
        khat, tA, tB, lw, kt, bb, csS, E1, E2, E3, E4, kap, bet, gam, rho = [T256(n) for n in
            ("khat", "tA", "tB", "lw", "kt", "bb", "csS", "E1", "E2", "E3", "E4", "kap", "bet", "gam", "rho")]
        s1 = T256("s1")
        sg = S.sb([128, 512], name="sg")
        stt_ = S.sb([128, 16], name="stt_")
        lT = S.sb([128, 3, 128], name="lT")
        kapT = S.sb([64, 4, 128], name="kapT")
        betT = S.sb([64, 4, 128], name="betT")
        gamT = S.sb([64, 4, 128], name="gamT")
        def T4(name):
            return S.sb([128, 4, 128], name=name)
        Zb = [T4("Zb%d" % i) for i in range(2)]
        ZTb = [T4("ZTb%d" % i) for i in range(2)]
        G = T4("G")
        LgT = T4("LgT")
        MgT = T4("MgT")
        X1 = S.sb([128, 4, 64], name="X1")
        NSQ = 2
        sq = []
        for i in range(NSQ):
            sq.append(dict(
                W1T=S.sb([64, 4, 128], name="W1T%d" % i), W2=S.sb([128, 4, 64], name="W2%d" % i),
                rhoT=S.sb([64, 4, 128], name="rhoT%d" % i), MbT=T4("MbT%d" % i), Y0=S.sb([128, 4, 64], name="Y0%d" % i),
                betp=T256("betp%d" % i), gamp=T256("gamp%d" % i), vq=T256("vq%d" % i),
                PC=S.sb([64, 4, 2], name="PC%d" % i), U=[S.sb([128, 4, 64], name="U%d_%d" % (i, b_)) for b_ in range(2)],
                gampm=S.sb([128, 2, 256], name="gampm%d" % i)))
            for b_ in range(2):
                S.memset("pool", sq[i]["U"][b_][:], 0.0)
        pm = [S.ps([128, 512], name="pm%d" % i) for i in range(3)]
        pbig = [S.ps([128, 4, 128], name="pbig%d" % i) for i in range(2)]
        pU = S.ps([128, 4, 64], name="pU")
        pY = S.ps([128, 4, 64], name="pY")
        pA = S.ps([64, 4, 64], name="pA")
        cnt = {"pm": 0, "pbig": 0}
        def npm():
            cnt["pm"] += 1
            return pm[cnt["pm"] % 3]
        def npb():
            cnt["pbig"] += 1
            return pbig[cnt["pbig"] % 2]
        def bc4(v):
            return vw(v, lambda a: a.unsqueeze(1).to_broadcast([128, 4, 128]))
        def bh(v, n=64):
            return vw(v, lambda a: a.unsqueeze(2).to_broadcast([a.shape[0], 4, n]))

        def unit(i, d, Q, first):
            r0 = i * 128
            S.dma("sp", pt[:], dv(Pr, lambda a: a[r0:r0 + 128, :]))
            if i in (0, 2):
                S.memset("pool", prv[:], 0.0)
                S.dma("pool", prv[1:128, :], dv(Pr, lambda a: a[r0:r0 + 127, :]))
            else:
                S.dma("pool", prv[:], dv(Pr, lambda a: a[r0 - 1:r0 + 127, :]))
            if i in (1, NTB - 1):
                S.memset("pool", nx[:], 0.0)
                S.dma("act", nx[0:127, :], dv(Pr, lambda a: a[r0 + 1:r0 + 128, :]))
            else:
                S.dma("act", nx[:], dv(Pr, lambda a: a[r0 + 1:r0 + 129, :]))
            S.tt("pool", prv[:], prv[:], pt[:], ALU.subtract)
            S.tt("pool", prv[:], prv[:], mup_b[:], ALU.mult)
            S.tt("dve", nx[:], nx[:], pt[:], ALU.subtract)
            S.tt("dve", nx[:], nx[:], mun_b[:], ALU.mult)
            S.tt("pool", zt[:], pt[:], prv[:], ALU.add)
            S.tt("dve", zt[:], zt[:], nx[:], ALU.add)
            r_ = zt[:, 0:256]
            k_ = zt[:, 256:512]
            v_ = zt[:, 512:768]
            S.tt("pool", khat[:], k_, kk_b, ALU.mult)
            S.tt("pool", tA[:], khat[:], khat[:], ALU.mult)
            S.reduce("dve", stt_[:, 0:4], h4(tA[:]), ALU.add)
            S.ts("dve", stt_[:, 4:8], stt_[:, 0:4], 1e-12, None, ALU.add)
            S.act(stt_[:, 8:12], stt_[:, 4:8], AF.Sqrt)
            S.recip(stt_[:, 12:16], stt_[:, 8:12])
            S.tt("dve", h4(khat[:]), h4(khat[:]), bh(stt_[:, 12:16]), ALU.mult)
            S.act(s1[:, 0:64], zt[:, 768:832], AF.Tanh)
            if first:
                S.act(s1[:, 128:256], zt[:, 896:1024], AF.Sigmoid)
            p_ = npm()
            pv3 = vw(p_[:, 0:384], lambda a: a.rearrange("p (c n) -> p c n", c=3))
            S.tr(vw(pv3, lambda a: a[0:64, 0, :]), s1[:, 0:64], ident)
            S.tr(vw(pv3, lambda a: a[0:64, 1, :]), zt[:, 832:896], ident)
            if first:
                S.tr(vw(pv3, lambda a: a[:, 2, :]), s1[:, 128:256], ident)
                S.copy("act", lT[:, 2, :], vw(pv3, lambda a: a[:, 2, :]))
            S.copy("dve", lT[0:64, 0:2, :], vw(pv3, lambda a: a[0:64, 0:2, :]))
            p_ = npm()
            S.mm(p_[:, 0:256], lT[0:64, 0, :], wup_s[:, d * 256:(d + 1) * 256])
            S.mm(p_[:, 256:512], lT[0:64, 1, :], aup_s[:, d * 256:(d + 1) * 256])
            S.tt("dve", sg[:], p_[:], w0a0_b[:, d * 512:(d + 1) * 512], ALU.add)
            S.act(sg[:], sg[:], AF.Sigmoid)
            S.ts("pool", lw[:], sg[:, 0:256], -DECAY_SCALE, None, ALU.mult)
            a_ = sg[:, 256:512]
            S.tt("pool", tA[:], a_, ka_b, ALU.mult)
            S.tt("pool", tA[:], tA[:], oka_b[:], ALU.add)
            S.tt("pool", kt[:], k_, tA[:], ALU.mult)
            S.tt("dve", bb[:], a_, khat[:], ALU.mult)
            if first:
                p_ = npm()
                S.mm(p_[:, 0:256], lT[:, 2, :], gup_s[:])
                S.copy("act", gS[:, i, :], p_[:, 0:256])
                S.copy("pool", vS[:, i, :], v_)
            S.copy("pool", Q["vq"][:], v_)
            S.tt("pool", tB[:], r_, kt[:], ALU.mult)
            S.tt("pool", tB[:], tB[:], rk_b, ALU.mult)
            S.reduce("dve", bon[:, i, d * 4:(d + 1) * 4], h4(tB[:]), ALU.add)
            if DBG["stage"] < 1:
                return
            p_ = npm()
            S.mm(p_[:, 0:256], cst[:, 2 + d, :], lw[:])
            S.mm(p_[:, 256:512], same, lw[:])
            S.act(E1[:], p_[:, 0:256], AF.Exp)
            S.act(E2[:], p_[:, 0:256], AF.Exp, scale=-1.0)
            S.copy("act", csS[:], p_[:, 0:256])
            S.tt("dve", tA[:], p_[:, 0:256], lw[:], ALU.subtract)
            S.act(E3[:], tA[:], AF.Exp)
            S.tt("dve", tB[:], p_[:, 256:512], csS[:], ALU.subtract)
            S.act(E4[:], tB[:], AF.Exp)
            S.tt("pool", kap[:], khat[:], E3[:], ALU.mult)
            S.tt("dve", bet[:], bb[:], E2[:], ALU.mult)
            S.tt("pool", gam[:], kt[:], E2[:], ALU.mult)
            S.tt("dve", rho[:], r_, E1[:], ALU.mult)
            S.tt("pool", Q["betp"][:], bb[:], E4[:], ALU.mult)
            S.tt("pool", Q["gamp"][:], kt[:], E4[:], ALU.mult)
            for b_ in range(2):
                S.ts("pool", Q["gampm"][:, b_, :], Q["gamp"][:], vw(cst[:, 4, :], lambda a: a[:, 64 * b_:64 * b_ + 1]), None, ALU.mult)
            p_ = npm()
            ppc = vw(p_[0:64, 0:8], lambda a: a.rearrange("p (h c) -> p h c", h=4))
            for h in range(4):
                S.mm(vw(ppc, lambda a: a[:, h, :]), lw[:, h * 64:(h + 1) * 64], blockind)
            S.act(Q["PC"][:], ppc, AF.Exp)
            if DBG["stage"] < 2:
                return
            for (src, dst) in ((kap, kapT), (bet, betT), (gam, gamT), (rho, Q["rhoT"])):
                p_ = npm()
                p4 = vw(p_[0:64, :], lambda a: a.rearrange("p (h n) -> p h n", h=4))
                for h in range(4):
                    S.tr(vw(p4, lambda a: a[:, h, :]), src[:, h * 64:(h + 1) * 64], ident)
                S.copy("act" if dst in (kapT, gamT) else "dve", dst[:], p4)
            rhoT = Q["rhoT"]
            mS = bc4(cst[:, d, :])
            mST = bc4(cst[:, 1 - d, :])
            mI = bc4(cst[:, 2 + d, :])
            if DBG["stage"] < 3:
                return
            Z = Zb[0]
            ZT = ZTb[0]
            p_ = npb()
            for h in range(4):
                S.mm(p_[:, h, :], betT[:, h, :], kapT[:, h, :])
            S.stt("dve", Z[:], p_[:], -1.0, mS, ALU.mult, ALU.mult)
            p_ = npb()
            for h in range(4):
                S.mm(p_[:, h, :], kapT[:, h, :], betT[:, h, :])
            S.stt("dve", ZT[:], p_[:], -1.0, mST, ALU.mult, ALU.mult)
            S.tt("pool", G[:], Z[:], bc4(ident), ALU.add)
            for lev in range(5):
                Zn = Zb[(lev + 1) % 2]
                ZTn = ZTb[(lev + 1) % 2]
                if lev < 4:
                    p_ = npb()
                    for h in range(4):
                        S.mm(p_[:, h, :], ZT[:, h, :], Z[:, h, :])
                    S.copy("act", Zn[:], p_[:])
                p_ = npb()
                for h in range(4):
                    S.mm(p_[:, h, :], Z[:, h, :], ZT[:, h, :])
                S.copy("dve", ZTn[:], p_[:])
                p_ = npb()
                for h in range(4):
                    S.mm(p_[:, h, :], ZTn[:, h, :], G[:, h, :])
                S.tt("dve", G[:], G[:], p_[:], ALU.add)
                Z, ZT = Zn, ZTn
            if DBG["stage"] < 4:
                return
            p_ = npb()
            for h in range(4):
                S.mm(p_[:, h, :], gamT[:, h, :], kapT[:, h, :])
            S.tt("dve", LgT[:], p_[:], mS, ALU.mult)
            p_ = npm()
            p4 = vw(p_[:, 0:256], lambda a: a.rearrange("p (h n) -> p h n", h=4))
            for h in range(4):
                S.mm(vw(p4, lambda a: a[:, h, :]), LgT[:, h, :], zt[:, 512 + h * 64:512 + (h + 1) * 64])
            S.copy("act", X1[:], p4)
            p_ = npm()
            p4 = vw(p_[:, 0:256], lambda a: a.rearrange("p (h n) -> p h n", h=4))
            for h in range(4):
                S.mm(vw(p4, lambda a: a[:, h, :]), G[:, h, :], X1[:, h, :])
            S.copy("act", Q["W2"][:], p4)
            p_ = npm()
            p4 = vw(p_[0:64, :], lambda a: a.rearrange("p (h n) -> p h n", h=4))
            for h in range(4):
                S.mm(vw(p4, lambda a: a[:, h, :]), kap[:, h * 64:(h + 1) * 64], G[:, h, :])
            S.copy("dve", Q["W1T"][:], p4)
            p_ = npb()
            for h in range(4):
                S.mm(p_[:, h, :], betT[:, h, :], rhoT[:, h, :])
            S.tt("dve", Q["MbT"][:], p_[:], mI, ALU.mult)
            p_ = npb()
            for h in range(4):
                S.mm(p_[:, h, :], gamT[:, h, :], rhoT[:, h, :])
            S.tt("dve", MgT[:], p_[:], mI, ALU.mult)
            p_ = npm()
            p4 = vw(p_[:, 0:256], lambda a: a.rearrange("p (h n) -> p h n", h=4))
            for h in range(4):
                S.mm(vw(p4, lambda a: a[:, h, :]), MgT[:, h, :], zt[:, 512 + h * 64:512 + (h + 1) * 64])
            S.copy("act", Q["Y0"][:], p4)
            S.drain_bg()
            S.bg = seqgen(i, d, Q)

        def seqgen(i, d, Q):
            Ad = A[d]
            rhoT = Q["rhoT"]
            for blk in ((0, 1) if d == 0 else (1, 0)):
                p0 = blk * 64
                ps_ = slice(p0, p0 + 64)
                U = Q["U"][blk]
                for h in range(4):
                    S.mm(pU[:, h, :], Q["W1T"][:, h, :], Ad[:, h, :])
                yield
                S.stt("dve", U[ps_, :, :], pU[ps_, :, :], -1.0, Q["W2"][ps_, :, :], ALU.mult, ALU.subtract)
                yield
                for h in range(4):
                    S.mm(pY[:, h, :], rhoT[:, h, :], Ad[:, h, :], start=True, stop=False)
                    S.mm(pY[:, h, :], Q["MbT"][:, h, :], U[:, h, :], start=False, stop=True)
                yield
                for h in range(4):
                    S.mm(pA[:, h, :], Q["betp"][:, h * 64:(h + 1) * 64], U[:, h, :], start=True, stop=False)
                    S.mm(pA[:, h, :], Q["gampm"][:, blk, h * 64:(h + 1) * 64], Q["vq"][:, h * 64:(h + 1) * 64], start=False, stop=True)
                yield
                S.tt("pool", Ad[:], Ad[:], bh(vw(Q["PC"][:], lambda a: a[:, :, blk])), ALU.mult)
                yield
                S.tt("dve", Ad[:], Ad[:], pA[:], ALU.add)
                yield
                S.tt("dve", h4(Yd[d][ps_, i, :]), pY[ps_, :, :], Q["Y0"][ps_, :, :], ALU.add)
                yield

        fwd_order = list(range(NTB))
        rev_order = [1, 0] + list(range(NTB - 1, 1, -1))
        nu = 0
        for s in range(NTB):
            if not (CTX.get("own_only", False) and fwd_order[s] >= 10):
                unit(fwd_order[s], 0, sq[nu % NSQ], True)
                nu += 1
            unit(rev_order[s], 1, sq[nu % NSQ], False)
            nu += 1
        S.drain_bg()
        y = T256("ry")
        yc = T256("ryc")
        ot = [T256("rot%d" % i) for i in range(2)]
        rst = S.sb([128, 24], name="rst")
        for i in range(NTB):
            if CTX.get("own_only", False) and i >= 10:
                continue
            S.tt("dve", y[:], Yd[0][:, i, :], Yd[1][:, i, :], ALU.add)
            S.reduce("dve", rst[:, 0:4], h4(y[:]), ALU.add)
            S.ts("dve", rst[:, 4:8], rst[:, 0:4], 1.0 / 64, None, ALU.mult)
            S.tt("dve", h4(yc[:]), h4(y[:]), bh(rst[:, 4:8]), ALU.subtract)
            S.tt("pool", y[:], yc[:], yc[:], ALU.mult)
            S.reduce("dve", rst[:, 8:12], h4(y[:]), ALU.add)
            S.ts("dve", rst[:, 12:16], rst[:, 8:12], 1.0 / 64, GN_EPS, ALU.mult, ALU.add)
            S.act(rst[:, 16:20], rst[:, 12:16], AF.Sqrt)
            S.recip(rst[:, 20:24], rst[:, 16:20])
            S.tt("dve", h4(yc[:]), h4(yc[:]), bh(rst[:, 20:24]), ALU.mult)
            S.tt("pool", yc[:], yc[:], gng_b, ALU.mult)
            S.tt("pool", yc[:], yc[:], gnb_b, ALU.add)
            S.tt("dve", rst[:, 0:4], bon[:, i, 0:4], bon[:, i, 4:8], ALU.add)
            S.tt("dve", h4(y[:]), h4(vS[:, i, :]), bh(rst[:, 0:4]), ALU.mult)
            S.tt("pool", yc[:], yc[:], y[:], ALU.add)
            o_ = ot[i % 2]
            S.tt("dve", o_[:], yc[:], gS[:, i, :], ALU.mult)
            S.dma(("sp", "act")[i % 2], dv(Or, lambda a: a[i * 128:(i + 1) * 128, :]), o_[:], is_output=True)
        barrier(S)
        print("Brwkv ninstr", S.ninstr)
    return nc


def rwkv_consts():
    idx = np.arange(128)
    blk = idx // 64
    same = blk[:, None] == blk[None, :]
    lt = idx[:, None] < idx[None, :]
    gt = idx[:, None] > idx[None, :]
    eye = np.eye(128, dtype=bool)
    c = np.stack([same & lt, same & gt, same & (lt | eye), same & (gt | eye), same, eye], 1).astype(np.float32)
    return np.ascontiguousarray(c.reshape(128, 6 * 128))


def rwkv_cols(g):
    c = []
    for j in range(3):
        c += list(range(512 * j + 256 * g, 512 * j + 256 * g + 256))
    c += list(range(1536, 1792))
    return np.array(c)


def run_Brwkv(Pfull, shift_prev, shift_next, decay_w0, decay_up, iclr_a0, iclr_up, gate_up, k_k, k_a, r_k, gn_g, gn_b):
    nc = get_nc("Brwkv", build_Brwkv)
    consts = rwkv_consts()
    maps = []
    for core in range(NCORES):
        b, g = core // 2, core % 2
        cols = rwkv_cols(g)
        hs = slice(256 * g, 256 * g + 256)
        w0a0 = np.concatenate([decay_w0[0, hs], iclr_a0[0, hs], decay_w0[1, hs], iclr_a0[1, hs]]).reshape(1, -1)
        wup = np.concatenate([decay_up[0][:, hs], decay_up[1][:, hs]], 1)
        aup = np.concatenate([iclr_up[0][:, hs], iclr_up[1][:, hs]], 1)
        vecs = np.concatenate([k_k[hs], k_a[hs], r_k[hs], gn_g[hs], gn_b[hs]]).reshape(1, -1)
        maps.append(dict(Pr=np.ascontiguousarray(Pfull[b][:, cols]), mup=np.ascontiguousarray(shift_prev[cols].reshape(1, -1)),
                         mun=np.ascontiguousarray(shift_next[cols].reshape(1, -1)), w0a0=np.ascontiguousarray(w0a0),
                         wup=np.ascontiguousarray(wup), aup=np.ascontiguousarray(aup), gup=np.ascontiguousarray(gate_up[:, hs]),
                         vecs=np.ascontiguousarray(vecs), consts=consts))
    res = run_bass_kernel_spmd(nc, maps, core_ids=list(range(NCORES)))
    return [r["Or"] for r in res.results]


NEXP = 32
TG = [(0, 512), (512, 512), (1024, 128)]


def build_C(final):
    nc = CTX["nc"]
    io = CTX["io"]
    xin = io["xin"]
    Oin = io["Oin"]
    mod = io["mod"]
    w_out = io["w_out"]
    g2 = io["g2"]
    wr = io["wr"]
    br = io["br"]
    w1 = io["w1"]
    w3 = io["w3"]
    w2 = io["w2"]
    gf = io["gf"]
    ident = io["ident"]
    xout = io["xout"]
    with ExitStack() as st:
        S = CTX["S"]
        S.stack = st
        idt = S.sb([128, 128], name="idt")
        S.dma("sp", idt[:], ident)
        idb = S.sb([128, 128], BF16, name="idb")
        S.copy("dve", idb[:], idt[:])
        xres = S.sb([128, NT, D], name="xres")
        for i in range(NT):
            S.dma(("sp", "act")[i % 2], xres.k(i, (slice(None), i, slice(None))), dv(xin, lambda a: a[i * 128:(i + 1) * 128, :]))
        def modb(r, j, name, stack, q="sp"):
            t = S.sb([128, D], name=name, stack=stack)
            S.dma(q, t[:], dv(mod, lambda a: a[r:r + 1, j * D:(j + 1) * D].partition_broadcast(128)))
            return t
        def setof(i):
            return 1 if i == 0 else 0
        with ExitStack() as st1:
            gate1 = [modb(r, 2, "gate1_%d" % r, st1, ("sp", "act")[r]) for r in range(2)]
            OT = S.sb([128, NT, 16, 128], BF16, name="OT", stack=st1)
            ot_f = [S.sb([128, D], name="ot_f%d" % i, stack=st1) for i in range(2)]
            ot_b = S.sb([128, D], BF16, name="ot_b", stack=st1)
            ptr = [S.ps([128, 4, 128], BF16, name="ptrC%d" % i, stack=st1) for i in range(2)]
            for i in range(NT):
                o_ = ot_f[i % 2]
                S.dma(("sp", "pool")[i % 2], o_[:], dv(Oin, lambda a: a[i * 128:(i + 1) * 128, :]))
                S.copy("pool", ot_b[:], o_[:])
                for q in range(4):
                    p_ = ptr[q % 2]
                    for c4 in range(4):
                        c = q * 4 + c4
                        S.tr(p_[:, c4, :], ot_b[:, c * 128:(c + 1) * 128], idb[:])
                    S.copy(("dve", "act")[q % 2], OT[:, i, q * 4:q * 4 + 4, :], p_[:])
            wst = [S.sb([128, 4, 512], name="wstC%d" % i, stack=st1) for i in range(3)]
            wbf = [S.sb([128, 16, 512], BF16, name="wbfC%d" % i, stack=st1) for i in range(2)]
            po = [S.ps([128, 512], name="poC%d" % i, stack=st1) for i in range(3)]
            tmp = [S.sb([128, 512], name="tmpC%d" % i, stack=st1) for i in range(2)]
            k = 0
            n = 0
            for j in range(4):
                wb = wbf[j % 2]
                for q in range(4):
                    ws = wst[k % 3]
                    S.dma(("sp", "pool")[k % 2], ws[:],
                          dv(w_out, lambda a: a[q * 512:(q + 1) * 512, j * 512:(j + 1) * 512].rearrange("(c p) n -> p c n", p=128)))
                    S.copy(rr(k), wb.k(q, (slice(None), slice(q * 4, q * 4 + 4), slice(None))), ws[:])
                    k += 1
                for i in range(NT):
                    p_ = po[n % 3]
                    t_ = tmp[n % 2]
                    n += 1
                    for c in range(16):
                        S.mm(p_[:], OT[:, i, c, :], wb.k(c // 4, (slice(None), c, slice(None))), start=(c == 0), stop=(c == 15))
                    S.tt("dve", t_[:], p_[:], gate1[setof(i)][:, j * 512:(j + 1) * 512], ALU.mult)
                    xs = xres.k(i, (slice(None), i, slice(j * 512, (j + 1) * 512)))
                    S.tt("pool", xs, xs, t_[:], ALU.add)
            barrier(S)
        DC = 99
        h2T = S.sb([128, NT, 16, 128], BF16, name="h2T")
        comb = S.sb([128, NT, 32], name="comb")
        gate2 = [modb(r, 5, "gate2_%d" % r, st, ("sp", "act")[r]) for r in range(2)]
        with ExitStack() as st2:
            g2b = S.sb([128, D], name="g2b", stack=st2)
            S.dma("pool", g2b[:], dv(g2, lambda a: a.partition_broadcast(128)))
            gs = []
            sh = []
            for r in range(2):
                sh.append(modb(r, 3, "sh2_%d" % r, st2, "sp"))
                sc_ = modb(r, 4, "sc2_%d" % r, st2, "act")
                S.stt("dve", sc_[:], sc_[:], 1.0, g2b[:], ALU.add, ALU.mult)
                gs.append(sc_)
            wr_s = S.sb([128, 16, 36], name="wr_s", stack=st2)
            S.dma("sp", wr_s[:], dv(wr, lambda a: a.rearrange("(c p) n -> p c n", p=128)))
            br_b = S.sb([128, 36], name="br_b", stack=st2)
            S.dma("act", br_b[:], dv(br, lambda a: a.partition_broadcast(128)))
            junk = S.sb([128, D], name="junkC", stack=st2)
            h2 = S.sb([128, D], name="h2", stack=st2)
            h2T32 = S.sb([128, 16, 128], name="h2T32", stack=st2)
            stat = S.sb([128, NT, 4], name="statC", stack=st2)
            ptf = [S.ps([128, 4, 128], name="ptf%d" % i, stack=st2) for i in range(2)]
            plog = S.ps([128, 36], name="plog", stack=st2)
            rt = [S.sb([128, 128], name="rt%d" % i, stack=st2) for i in range(2)]
            for i in range(NT if DC >= 2 else 0):
                r = setof(i)
                xi = xres.k(i, (slice(None), i, slice(None)))
                S.act(junk[:], xi, AF.Square, accum=stat[:, i, 0:1])
                rstd_of(S, stat[:, i, 1:2], stat[:, i, 0:1], D, 1e-6, (stat[:, i, 2:3], stat[:, i, 3:4]))
                S.stt("dve", junk[:], xi, stat[:, i, 1:2], gs[r][:], ALU.mult, ALU.mult)
                S.tt("pool", h2[:], junk[:], sh[r][:], ALU.add)
                C2 = 99
                if C2 < 1:
                    continue
                for q in range(4):
                    p_ = ptf[q % 2]
                    for c4 in range(4):
                        c = q * 4 + c4
                        S.tr(p_[:, c4, :], h2[:, c * 128:(c + 1) * 128], idt[:])
                    S.copy("act", h2T32[:, q * 4:q * 4 + 4, :], p_[:])
                    S.copy("dve", h2T[:, i, q * 4:q * 4 + 4, :], p_[:])
                if C2 < 2:
                    continue
                for c in range(16):
                    S.mm(plog[:], h2T32[:, c, :], wr_s[:, c, :], start=(c == 0), stop=(c == 15))
                R = rt[i % 2]
                S.tt("dve", R[:, 0:36], plog[:], br_b[:], ALU.add)
                if C2 < 3:
                    continue
                S.reduce("dve", R[:, 36:37], R[:, 0:4], ALU.max)
                S.ts("dve", R[:, 37:38], R[:, 36:37], -1.0, None, ALU.mult)
                S.ts("dve", R[:, 40:44], R[:, 0:4], R[:, 36:37], None, ALU.is_equal)
                S.act(R[:, 44:48], R[:, 0:4], AF.Exp, bias=R[:, 37:38], accum=R[:, 38:39])
                S.recip(R[:, 39:40], R[:, 38:39])
                S.tt("dve", vw(R[:, 48:80], lambda a: a.rearrange("p (g e) -> p g e", g=4)),
                     vw(R[:, 4:36], lambda a: a.rearrange("p (g e) -> p g e", g=4)),
                     vw(R[:, 40:44], lambda a: a.unsqueeze(2).to_broadcast([128, 4, 8])), ALU.mult)
                S.reduce("dve", R[:, 80:88], vw(R[:, 48:80], lambda a: a.rearrange("p (g e) -> p e g", g=4)), ALU.add)
                S.reduce("dve", R[:, 88:89], R[:, 80:88], ALU.max)
                S.ts("dve", R[:, 89:97], R[:, 80:88], R[:, 88:89], None, ALU.is_equal)
                S.stt("dve", R[:, 97:105], R[:, 89:97], -1e30, R[:, 80:88], ALU.mult, ALU.add)
                S.reduce("dve", R[:, 105:106], R[:, 97:105], ALU.max)
                S.ts("dve", R[:, 106:114], R[:, 97:105], R[:, 105:106], None, ALU.is_equal)
                S.tt("dve", R[:, 114:115], R[:, 105:106], R[:, 88:89], ALU.subtract)
                S.act(R[:, 115:116], R[:, 114:115], AF.Exp)
                S.ts("dve", R[:, 116:117], R[:, 115:116], 1.0, None, ALU.add)
                S.recip(R[:, 117:118], R[:, 116:117])
                S.tt("dve", R[:, 118:119], R[:, 115:116], R[:, 117:118], ALU.mult)
                S.ts("dve", R[:, 119:127], R[:, 89:97], R[:, 117:118], None, ALU.mult)
                S.stt("dve", R[:, 119:127], R[:, 106:114], R[:, 118:119], R[:, 119:127], ALU.mult, ALU.add)
                S.tt("dve", vw(R[:, 48:80], lambda a: a.rearrange("p (g e) -> p g e", g=4)),
                     vw(R[:, 40:44], lambda a: a.unsqueeze(2).to_broadcast([128, 4, 8])),
                     vw(R[:, 119:127], lambda a: a.unsqueeze(1).to_broadcast([128, 4, 8])), ALU.mult)
                S.ts("dve", comb[:, i, :], R[:, 48:80], R[:, 39:40], None, ALU.mult)
            barrier(S)
        stg = [S.sb([128, 8, 256], name="stg%d" % i) for i in range(3)]
        w13b = S.sb([128, 2, 16, 256], BF16, name="w13b")
        w2b = [S.sb([128, 2, 2048], BF16, name="w2b%d" % i) for i in range(2)]
        actT = [S.sb([128, 2, TOK], BF16, name="actT%d" % i) for i in range(2)]
        sil = [S.sb([128, 512], name="sil%d" % i) for i in range(2)]
        tmpe = [S.sb([128, 512], name="tmpe%d" % i) for i in range(2)]
        pab = [S.ps([128, 512], name="pab%d" % i) for i in range(4)]
        pdn = [S.ps([128, 512], name="pdn%d" % i) for i in range(3)]
        ks = 0
        nab = 0
        nd = 0
        def load_expert(e):
            nonlocal ks
            for wi, wsrc in enumerate((w1, w3)):
                for hf in range(2):
                    s_ = stg[ks % 3]
                    S.dma("sp", s_[:],
                          dv(wsrc, lambda a: a[e, hf * 1024:(hf + 1) * 1024, :].rearrange("(c p) n -> p c n", p=128)))
                    S.copy(rr(ks), w13b.k((wi, hf), (slice(None), wi, slice(hf * 8, hf * 8 + 8), slice(None))), s_[:])
                    ks += 1
            w2n = w2b[e % 2]
            for hc in range(2):
                s_ = stg[ks % 3]
                S.dma("sp", vw(s_[:], lambda a: a.rearrange("p c n -> p (c n)")),
                      dv(w2, lambda a: a[e, hc * 128:(hc + 1) * 128, :]))
                S.copy(rr(ks), w2n.k(hc, (slice(None), hc, slice(None))), vw(s_[:], lambda a: a.rearrange("p c n -> p (c n)")))
                ks += 1
        load_expert(0)
        for e in range(NEXP):
            w2_ = w2b[e % 2]
            aT = actT[e % 2]
            for hc in range(2):
                for (t0, tn) in TG:
                    pa = pab[nab % 4]
                    pb = pab[(nab + 1) % 4]
                    nab += 2
                    i0 = t0 // 128
                    ni = tn // 128
                    rhs_ = lambda c: vw(h2T[:, i0:i0 + ni, c, :], lambda a: a)
                    for c in range(16):
                        S.mm(vw(pa[:, 0:tn], lambda a: a.rearrange("p (i t) -> p i t", i=ni)),
                             w13b.k((0, c // 8), (slice(None), 0, c, slice(hc * 128, (hc + 1) * 128))), rhs_(c),
                             start=(c == 0), stop=(c == 15))
                    for c in range(16):
                        S.mm(vw(pb[:, 0:tn], lambda a: a.rearrange("p (i t) -> p i t", i=ni)),
                             w13b.k((1, c // 8), (slice(None), 1, c, slice(hc * 128, (hc + 1) * 128))), rhs_(c),
                             start=(c == 0), stop=(c == 15))
                    s_ = sil[(nab // 2) % 2]
                    S.act(s_[:, 0:tn], pa[:, 0:tn], AF.Silu)
                    S.tt("dve", aT[:, hc, t0:t0 + tn], s_[:, 0:tn], pb[:, 0:tn], ALU.mult)
            if e + 1 < NEXP:
                load_expert(e + 1)
            for i in range(NT):
                for j in range(4):
                    p_ = pdn[nd % 3]
                    t_ = tmpe[nd % 2]
                    nd += 1
                    for hc in range(2):
                        S.mm(p_[:], aT[:, hc, i * 128:(i + 1) * 128], w2_.k(hc, (slice(None), hc, slice(j * 512, (j + 1) * 512))),
                             start=(hc == 0), stop=(hc == 1))
                    S.stt("dve", t_[:], p_[:], comb[:, i, e:e + 1], gate2[setof(i)][:, j * 512:(j + 1) * 512], ALU.mult, ALU.mult)
                    xs = xres.k(i, (slice(None), i, slice(j * 512, (j + 1) * 512)))
                    S.tt("pool", xs, xs, t_[:], ALU.add)
        if final:
            gfb = S.sb([128, 512], name="gfb")
            fstat = S.sb([128, NT, 4], name="fstat")
            for i in range(NT):
                xi = xres.k(i, (slice(None), i, slice(None)))
                for j in range(4):
                    S.act(sil[j % 2][:], xres.k(i, (slice(None), i, slice(j * 512, (j + 1) * 512))), AF.Square,
                          accum=fstat[:, i, j:j + 1])
            fs2 = S.sb([128, NT, 4], name="fs2")
            for i in range(NT):
                S.reduce("dve", fs2[:, i, 0:1], fstat[:, i, :], ALU.add)
                rstd_of(S, fs2[:, i, 1:2], fs2[:, i, 0:1], D, 1e-6, (fs2[:, i, 2:3], fs2[:, i, 3:4]))
            for j in range(4):
                S.dma("sp", gfb[:], dv(gf, lambda a: a[:, j * 512:(j + 1) * 512].partition_broadcast(128)))
                for i in range(NT):
                    xs = xres.k(i, (slice(None), i, slice(j * 512, (j + 1) * 512)))
                    S.stt("dve", xs, xs, fs2[:, i, 1:2], gfb[:], ALU.mult, ALU.mult)
        for i in range(NT):
            S.dma(("sp", "act")[i % 2], dv(xout, lambda a: a[i * 128:(i + 1) * 128, :]), xres.k(i, (slice(None), i, slice(None))), is_output=True)
        barrier(S)
        print("C ninstr", S.ninstr)
    return nc


def run_C(xtok, Otok, mods, w_out_l, g2_l, gw, gb, ew, eb, w1_l, w3_l, w2_l, gf, final):
    nc = get_nc("C%d" % int(final), lambda: build_C(final))
    ident = np.eye(128, dtype=np.float32)
    wr = np.ascontiguousarray(np.concatenate([gw, ew], 1))
    br = np.ascontiguousarray(np.concatenate([gb, eb]).reshape(1, -1))
    maps = []
    for core in range(NCORES):
        maps.append(dict(xin=xtok[core], Oin=Otok[core], mod=mods[core], w_out=w_out_l, g2=g2_l.reshape(1, -1), wr=wr, br=br,
                         w1=w1_l, w3=w3_l, w2=w2_l, gf=gf.reshape(1, -1), ident=ident))
    res = run_bass_kernel_spmd(nc, maps, core_ids=list(range(NCORES)))
    return [r["xout"] for r in res.results]


def dcopy(S, dst, src, rows, n=0, chunk=256):
    qs = ("sp", "act", "pool")
    for k, r0 in enumerate(range(0, rows, chunk)):
        r1 = min(rows, r0 + chunk)
        S.dma(qs[(n + k) % 3], dv(dst, lambda a: a[r0:r1]), dv(src, lambda a: a[r0:r1]))


def rows_v(v):
    return [(128 * v, 128, 0), (256 + 1024 * v, 1024, 128)]


def build_fused():
    nc = bass.Bass("TRN2", target_bir_lowering=False)
    E = {}
    def ein(name, shape):
        E[name] = dram_in(nc, name, shape)
        return E[name]
    def scr(name, shape):
        return V(nc.dram_tensor(name, list(shape), F32).ap(), ("dram_" + name,))
    ein("x_seq", [TB, D])
    ein("cT", [128, 32])
    ein("ident", [128, 128])
    ein("consts", [128, 768])
    ein("cosG", [2048, 64]); ein("sinG", [2048, 64]); ein("cosM", [2048, 32]); ein("sinM", [2048, 32])
    ein("gf", [1, D])
    for l in range(2):
        ein("mod_w%d" % l, [D, 6 * D]); ein("mod_b%d" % l, [1, 6 * D]); ein("g1_%d" % l, [1, D]); ein("w_in%d" % l, [D, DIN])
        ein("w_out%d" % l, [D, D]); ein("g2_%d" % l, [1, D]); ein("wr%d" % l, [D, 36]); ein("br%d" % l, [1, 36])
        ein("w1_%d" % l, [NEXP, D, 256]); ein("w3_%d" % l, [NEXP, D, 256]); ein("w2_%d" % l, [NEXP, 256, D])
        ein("gq%d" % l, [1, 128]); ein("gk%d" % l, [1, 128]); ein("gmq%d" % l, [1, 384]); ein("gmkv%d" % l, [1, 256])
        for g in range(2):
            t = "%d%d" % (l, g)
            ein("wuq" + t, [384, 384]); ein("wukv" + t, [256, 512])
            ein("mup" + t, [1, 1024]); ein("mun" + t, [1, 1024]); ein("w0a0" + t, [1, 1024])
            ein("wup" + t, [64, 512]); ein("aup" + t, [64, 512]); ein("gup" + t, [128, 256]); ein("vecs" + t, [1, 1280])
    xout = dram_out(nc, "xout", [TOK, D])
    xin_v = scr("xin_v", [TOK, D]); P_v = scr("P_v", [TOK, DIN]); P_scr = scr("P_scr", [TB, DIN])
    Pa_scr = scr("Pa_scr", [TB, PA_W]); Pr_scr = scr("Pr_scr", [TB, 1024]); Oa_scr = scr("Oa_scr", [TB, 768])
    Or_scr = scr("Or_scr", [TB, 256]); O_scr = scr("O_scr", [TB, D]); Oin_v = scr("Oin_v", [TOK, D])
    xout_v = scr("xout_v", [TOK, D]); x1_scr = scr("x1_scr", [TB, D])
    mod_scr = [scr("mod_scr%d" % l, [2, 6 * D]) for l in range(2)]
    with ExitStack() as st0:
        S = Sched(nc, st0)
        CTX["nc"] = nc
        CTX["S"] = S
        def cs(v_, r0, r1, c0, c1):
            return dv(v_, lambda a: a[r0:r1, c0:c1])
        for l in range(2):
            xsrc = E["x_seq"] if l == 0 else x1_scr
            for v in range(2):
                S.prefix = "A%d%d_" % (l, v)
                for (s0, n, d0) in rows_v(v):
                    dcopy(S, cs(xin_v, d0, d0 + n, 0, D), cs(xsrc, s0, s0 + n, 0, D), n)
                CTX["do_mod"] = (v == 0)
                CTX["io"] = dict(xin=xin_v, cT=E["cT"], mod_w=E["mod_w%d" % l], mod_b=E["mod_b%d" % l], g1=E["g1_%d" % l],
                                 w_in=E["w_in%d" % l], ident=E["ident"], P=P_v, mod=mod_scr[l])
                build_A()
                for (s0, n, d0) in rows_v(v):
                    dcopy(S, cs(P_scr, s0, s0 + n, 0, DIN), cs(P_v, d0, d0 + n, 0, DIN), n)
            CTX["own_only"] = (l == 1)
            for g in range(2):
                t = "%d%d" % (l, g)
                S.prefix = "R%s_" % t
                segs = [(256 * g, 256), (512 + 256 * g, 256), (1024 + 256 * g, 256), (1536, 256)]
                c0 = 0
                for k, (sc0, w) in enumerate(segs):
                    dcopy(S, cs(Pr_scr, 0, TB, c0, c0 + w), cs(P_scr, 0, TB, sc0, sc0 + w), TB, n=k)
                    c0 += w
                CTX["io"] = dict(Pr=Pr_scr, mup=E["mup" + t], mun=E["mun" + t], w0a0=E["w0a0" + t], wup=E["wup" + t],
                                 aup=E["aup" + t], gup=E["gup" + t], vecs=E["vecs" + t], consts=E["consts"], Or=Or_scr)
                build_Brwkv()
                dcopy(S, cs(O_scr, 0, TB, 256 * g, 256 * g + 256), Or_scr, TB)
                S.prefix = "T%s_" % t
                q0 = 1792
                m0 = 1792 + 1536
                segs = [(q0 + 512 * g, 512), (q0 + 1024 + 128 * g, 128), (q0 + 1280 + 128 * g, 128), (m0, 704)]
                c0 = 0
                for k, (sc0, w) in enumerate(segs):
                    dcopy(S, cs(Pa_scr, 0, TB, c0, c0 + w), cs(P_scr, 0, TB, sc0, sc0 + w), TB, n=k)
                    c0 += w
                CTX["io"] = dict(Pa=Pa_scr, gq=E["gq%d" % l], gk=E["gk%d" % l], gmq=E["gmq%d" % l], gmkv=E["gmkv%d" % l],
                                 wuq=E["wuq" + t], wukv=E["wukv" + t], cosG=E["cosG"], sinG=E["sinG"], cosM=E["cosM"],
                                 sinM=E["sinM"], ident=E["ident"], Oa=Oa_scr)
                build_Battn()
                dcopy(S, cs(O_scr, 0, TB, 512 + 512 * g, 512 + 512 * g + 512), cs(Oa_scr, 0, TB, 0, 512), TB)
                dcopy(S, cs(O_scr, 0, TB, 1536 + 256 * g, 1536 + 256 * g + 256), cs(Oa_scr, 0, TB, 512, 768), TB, n=1)
            for v in ((0, 1) if l == 0 else (0,)):
                S.prefix = "C%d%d_" % (l, v)
                for (s0, n, d0) in rows_v(v):
                    dcopy(S, cs(xin_v, d0, d0 + n, 0, D), cs(xsrc, s0, s0 + n, 0, D), n)
                    dcopy(S, cs(Oin_v, d0, d0 + n, 0, D), cs(O_scr, s0, s0 + n, 0, D), n, n=1)
                final = (l == 1)
                CTX["io"] = dict(xin=xin_v, Oin=Oin_v, mod=mod_scr[l], w_out=E["w_out%d" % l], g2=E["g2_%d" % l], wr=E["wr%d" % l],
                                 br=E["br%d" % l], w1=E["w1_%d" % l], w3=E["w3_%d" % l], w2=E["w2_%d" % l], gf=E["gf"],
                                 ident=E["ident"], xout=(xout if final else xout_v))
                build_C(final)
                if not final:
                    for (s0, n, d0) in rows_v(v):
                        dcopy(S, cs(x1_scr, s0, s0 + n, 0, D), cs(xout_v, d0, d0 + n, 0, D), n)
                    barrier(S)
        S.stack = st0
        S.finish()
        print("fused ninstr", S.ninstr)
    return nc


_FUSED = {}


def kernel(x, c, ctx, c_ctx, mod_w, mod_b, norm1_g, norm2_g, w_in, w_out, shift_prev, shift_next,
           decay_w0, decay_up, iclr_a0, iclr_up, gate_up, k_k, k_a, r_k, gn_g, gn_b, q_norm_g, k_norm_g,
           mla_q_norm_g, mla_w_uq, mla_kv_norm_g, mla_w_ukv, router_gw, router_gb, router_ew, router_eb,
           exp_w1, exp_w3, exp_w2, final_norm_g):
    f = lambda a: np.ascontiguousarray(np.asarray(a, dtype=np.float32))
    x, c, ctx, c_ctx = f(x), f(c), f(ctx), f(c_ctx)
    if "nc" not in _FUSED:
        _FUSED["nc"] = build_fused()
    nc = _FUSED["nc"]
    ident = np.eye(128, dtype=np.float32)
    consts = rwkv_consts()
    cG, sG, cM, sM = rope_tables()
    shared = dict(ident=ident, consts=consts, gf=f(final_norm_g).reshape(1, -1))
    for l in range(2):
        shared["mod_w%d" % l] = f(mod_w[l]); shared["mod_b%d" % l] = f(mod_b[l]).reshape(1, -1)
        shared["g1_%d" % l] = f(norm1_g[l]).reshape(1, -1); shared["w_in%d" % l] = f(w_in[l]); shared["w_out%d" % l] = f(w_out[l])
        shared["g2_%d" % l] = f(norm2_g[l]).reshape(1, -1)
        shared["wr%d" % l] = f(np.concatenate([router_gw[l], router_ew[l]], 1))
        shared["br%d" % l] = f(np.concatenate([router_gb[l], router_eb[l]])).reshape(1, -1)
        shared["w1_%d" % l] = f(exp_w1[l]); shared["w3_%d" % l] = f(exp_w3[l]); shared["w2_%d" % l] = f(exp_w2[l])
        shared["gq%d" % l] = f(q_norm_g[l]).reshape(1, -1); shared["gk%d" % l] = f(k_norm_g[l]).reshape(1, -1)
        shared["gmq%d" % l] = f(mla_q_norm_g[l]).reshape(1, -1); shared["gmkv%d" % l] = f(mla_kv_norm_g[l]).reshape(1, -1)
        for g in range(2):
            t = "%d%d" % (l, g)
            shared["wuq" + t] = f(np.concatenate([mla_w_uq[l][:, (2 * g + hh) * 192:(2 * g + hh + 1) * 192] for hh in range(2)], 1))
            shared["wukv" + t] = f(np.concatenate([mla_w_ukv[l][:, (2 * g + hh) * 256:(2 * g + hh + 1) * 256] for hh in range(2)], 1))
            hs = slice(256 * g, 256 * g + 256)
            shared["gup" + t] = f(gate_up[l][:, hs])
            shared["vecs" + t] = f(np.concatenate([k_k[l][hs], k_a[l][hs], r_k[l][hs], gn_g[l][hs], gn_b[l][hs]])).reshape(1, -1)
    maps = []
    for core in range(NCORES):
        b, h = core // 2, core % 2
        m = dict(shared)
        if h == 0:
            m["x_seq"] = f(np.concatenate([ctx[b], x[b]], 0))
            m["cosG"], m["sinG"], m["cosM"], m["sinM"] = cG, sG, cM, sM
        else:
            m["x_seq"] = f(np.concatenate([ctx[b][::-1], x[b][::-1]], 0))
            m["cosG"], m["sinG"], m["cosM"], m["sinM"] = f(cG[::-1]), f(sG[::-1]), f(cM[::-1]), f(sM[::-1])
        m["cT"] = cT_layout(c[b], c_ctx)
        d0, d1 = (0, 1) if h == 0 else (1, 0)
        for l in range(2):
            for g in range(2):
                t = "%d%d" % (l, g)
                cols = rwkv_cols(g)
                hs = slice(256 * g, 256 * g + 256)
                sp, sn = (shift_prev[l], shift_next[l]) if h == 0 else (shift_next[l], shift_prev[l])
                m["mup" + t] = f(np.asarray(sp)[cols]).reshape(1, -1)
                m["mun" + t] = f(np.asarray(sn)[cols]).reshape(1, -1)
                m["w0a0" + t] = f(np.concatenate([decay_w0[l][d0, hs], iclr_a0[l][d0, hs], decay_w0[l][d1, hs], iclr_a0[l][d1, hs]])).reshape(1, -1)
                m["wup" + t] = f(np.concatenate([decay_up[l][d0][:, hs], decay_up[l][d1][:, hs]], 1))
                m["aup" + t] = f(np.concatenate([iclr_up[l][d0][:, hs], iclr_up[l][d1][:, hs]], 1))
        maps.append(m)
    res = run_bass_kernel_spmd(nc, maps, core_ids=list(range(NCORES)))
    out = np.empty((4, 2048, D), np.float32)
    for core in range(NCORES):
        b, h = core // 2, core % 2
        y = res.results[core]["xout"][128:]
        if h == 0:
            out[b, 0:1024] = y
        else:
            out[b, 1024:2048] = y[::-1]
    return out
```

```python
import numpy as np
from contextlib import ExitStack
import concourse.bass as bass
import concourse.mybir as mybir
from concourse.bass_utils import run_bass_kernel_spmd

F32 = mybir.dt.float32
BF16 = mybir.dt.bfloat16
AF = mybir.ActivationFunctionType
ALU = mybir.AluOpType
AX = mybir.AxisListType


class V:
    __slots__ = ("ap", "keys")

    def __init__(self, ap, keys):
        self.ap = ap
        self.keys = keys


class Tl:
    def __init__(self, t, name, psum=False):
        self.t = t
        self.name = name
        self.bk = (("BANK", name),) if psum else ()

    def __getitem__(self, idx):
        return V(self.t[idx], (self.name,) + self.bk)

    def k(self, sub, idx):
        return V(self.t[idx], ((self.name, sub),) + self.bk)

    def ks(self, subs, idx):
        return V(self.t[idx], tuple((self.name, s) for s in subs) + self.bk)


class Ev:
    __slots__ = ("prod", "count", "clock", "eng")

    def __init__(self, prod, count, clock, eng):
        self.prod = prod
        self.count = count
        self.clock = clock
        self.eng = eng


class Sched:
    NDMA = 48

    def __init__(self, nc, stack):
        self.nc = nc
        self.stack = stack
        self.E = {"pe": nc.tensor, "dve": nc.vector, "act": nc.scalar, "pool": nc.gpsimd, "sp": nc.sync}
        self.NR = 8
        self.sem = {e: [stack.enter_context(nc.semaphore("s_%s%d" % (e, i))) for i in range(self.NR)] for e in self.E if e != "sp"}
        self.cnt = {e: 0 for e in self.E}
        self.clock = {e: {} for e in self.E}
        self.dsem = [stack.enter_context(nc.semaphore("d%d" % i)) for i in range(self.NDMA)]
        self.dcnt = [0] * self.NDMA
        self.dnext = 0
        self.last_w = {}
        self.readers = {}
        self.nt = 0
        self.out_evs = []
        self.ninstr = 0
        self.prefix = ""
        self.bg = None
        self.in_bg = False
        self.tickc = 0
        self.bgk = 6

    def sb(self, shape, dt=F32, name=None, stack=None):
        self.nt += 1
        name = self.prefix + (name or ("t%d" % self.nt))
        t = (stack or self.stack).enter_context(self.nc.sbuf_tensor(name, list(shape), dt))
        return Tl(t, name)

    def ps(self, shape, dt=F32, name=None, stack=None):
        self.nt += 1
        name = self.prefix + (name or ("p%d" % self.nt))
        esz = 2 if dt == BF16 else 4
        t = (stack or self.stack).enter_context(self.nc.psum_tensor(name, [128, 2048 // esz], dt))
        n = 1
        for d_ in shape[1:]:
            n *= d_
        ap = t[0:shape[0], 0:n]
        if len(shape) == 3:
            ap = ap.rearrange("p (a b) -> p a b", a=shape[1])
        return Tl(ap, name, psum=True)

    def _semof(self, prod, count):
        if isinstance(prod, str):
            return self.sem[prod][(count - 1) % self.NR], (count - 1) // self.NR + 1
        return self.dsem[prod[1]], count

    def _wait(self, eng, ev):
        ck = self.clock[eng]
        if ck.get(ev.prod, 0) >= ev.count:
            return
        sm, cv = self._semof(ev.prod, ev.count)
        self.E[eng].wait_ge(sm, cv)
        self.ninstr += 1
        for p, c in ev.clock.items():
            if ck.get(p, 0) < c:
                ck[p] = c
        if ck.get(ev.prod, 0) < ev.count:
            ck[ev.prod] = ev.count

    def _deps(self, eng, reads, writes, is_dma):
        for v in reads:
            for k in v.keys:
                if isinstance(k, tuple) and k[0] == "BANK":
                    continue
                w = self.last_w.get(k)
                if w is not None:
                    if (not is_dma) and w.eng == eng and w.prod == eng and eng == "pe":
                        continue
                    self._wait(eng, w)
        for v in writes:
            for k in v.keys:
                w = self.last_w.get(k)
                if w is not None:
                    if not (w.prod == eng and not is_dma):
                        self._wait(eng, w)
                for r in self.readers.get(k, ()):
                    if r.prod == eng and not is_dma:
                        continue
                    self._wait(eng, r)

    def _record(self, ev, reads, writes):
        for v in reads:
            for k in v.keys:
                if isinstance(k, tuple) and k[0] == "BANK":
                    continue
                self.readers.setdefault(k, []).append(ev)
        for v in writes:
            for k in v.keys:
                self.last_w[k] = ev
                self.readers[k] = []

    def op(self, eng, fn, reads, writes):
        bks = set()
        for v in list(reads) + list(writes):
            for k in v.keys:
                if isinstance(k, tuple) and k[0] == "BANK":
                    bks.add(k)
        if bks:
            writes = list(writes) + [V(None, tuple(bks))]
        self._deps(eng, reads, writes, False)
        ins = fn(self.E[eng])
        self.cnt[eng] += 1
        c = self.cnt[eng]
        ins.then_inc(self.sem[eng][(c - 1) % self.NR], 1)
        self.ninstr += 1
        ck = self.clock[eng]
        ev = Ev(eng, c, dict(ck), eng)
        ev.clock[eng] = c
        self._record(ev, reads, writes)
        self._tick()
        return ev

    def _tick(self):
        if self.bg is not None and not self.in_bg:
            self.tickc += 1
            if self.tickc % self.bgk == 0:
                self.in_bg = True
                try:
                    next(self.bg)
                except StopIteration:
                    self.bg = None
                self.in_bg = False

    def drain_bg(self):
        if self.bg is not None:
            self.in_bg = True
            for _ in self.bg:
                pass
            self.bg = None
            self.in_bg = False

    def dma(self, q, out, in_, is_output=False, **kw):
        self._deps(q, [in_], [out], True)
        s = self.dnext
        self.dnext = (self.dnext + 1) % self.NDMA
        prod = ("dma", s)
        prev = self.dcnt[s]
        if prev and self.clock[q].get(prod, 0) < prev:
            self.E[q].wait_ge(self.dsem[s], prev)
            self.clock[q][prod] = prev
        ins = self.E[q].dma_start(out=out.ap, in_=in_.ap, **kw)
        self.dcnt[s] += 16
        ins.then_inc(self.dsem[s], 16)
        self.ninstr += 1
        ev = Ev(prod, self.dcnt[s], dict(self.clock[q]), q)
        self._record(ev, [in_], [out])
        if is_output:
            self.out_evs.append(ev)
        return ev

    def coll(self, kind, ins, outs, groups):
        q = "pool"
        for o in outs:
            self._deps(q, ins, [o], True)
        s = self.dnext
        self.dnext = (self.dnext + 1) % self.NDMA
        prod = ("dma", s)
        prev = self.dcnt[s]
        if prev and self.clock[q].get(prod, 0) < prev:
            self.E[q].wait_ge(self.dsem[s], prev)
            self.clock[q][prod] = prev
        ins_ = self.E[q].collective_compute(kind, ALU.bypass, groups, [v.ap for v in ins], [v.ap for v in outs])
        self.dcnt[s] += 16
        ins_.then_inc(self.dsem[s], 16)
        self.ninstr += 1
        ev = Ev(prod, self.dcnt[s], dict(self.clock[q]), q)
        self._record(ev, ins, outs)
        return ev

    def finish(self):
        for ev in self.out_evs:
            self._wait("sp", ev)
        for e in self.E:
            if e != "sp" and self.cnt[e]:
                self._wait("sp", Ev(e, self.cnt[e], {}, e))

    def mm(self, out, lhsT, rhs, start=True, stop=True):
        return self.op("pe", lambda e: e.matmul(out.ap, lhsT.ap, rhs.ap, start=start, stop=stop), [lhsT, rhs], [out])

    def tr(self, out, in_, ident):
        return self.op("pe", lambda e: e.transpose(out.ap, in_.ap, ident.ap), [in_, ident], [out])

    def act(self, out, in_, func, bias=None, scale=None, accum=None, eng="act"):
        kw = {}
        rd = [in_]
        wr = [out]
        if bias is not None:
            if isinstance(bias, V):
                kw["bias"] = bias.ap
                rd.append(bias)
            else:
                kw["bias"] = bias
        if scale is not None:
            if isinstance(scale, V):
                kw["scale"] = scale.ap
                rd.append(scale)
            else:
                kw["scale"] = scale
        if accum is not None:
            kw["accum_out"] = accum.ap
            wr.append(accum)
        return self.op("act", lambda e: e.activation(out.ap, in_.ap, func, **kw), rd, wr)

    def tt(self, eng, out, a, b, op):
        return self.op(eng, lambda e: e.tensor_tensor(out.ap, a.ap, b.ap, op), [a, b], [out])

    def ts(self, eng, out, a, s1, s2, op0, op1=None, accum=None):
        rd = [a]
        wr = [out]
        a1 = s1
        a2 = s2
        if isinstance(s1, V):
            rd.append(s1)
            a1 = s1.ap
        if isinstance(s2, V):
            rd.append(s2)
            a2 = s2.ap
        kw = {}
        if op1 is not None:
            kw["op1"] = op1
        if accum is not None:
            kw["accum_out"] = accum.ap
            wr.append(accum)
        return self.op(eng, lambda e: e.tensor_scalar(out.ap, a.ap, a1, a2, op0, **kw), rd, wr)

    def stt(self, eng, out, a, s, b, op0, op1):
        rd = [a, b]
        sa = s
        if isinstance(s, V):
            rd.append(s)
            sa = s.ap
        return self.op(eng, lambda e: e.scalar_tensor_tensor(out.ap, a.ap, sa, b.ap, op0, op1), rd, [out])

    def copy(self, eng, out, in_):
        if eng == "act":
            return self.op("act", lambda e: e.copy(out.ap, in_.ap), [in_], [out])
        return self.op(eng, lambda e: e.tensor_copy(out.ap, in_.ap), [in_], [out])

    def memset(self, eng, out, val):
        return self.op(eng, lambda e: e.memset(out.ap, val), [], [out])

    def reduce(self, eng, out, in_, op, axis=AX.X):
        return self.op(eng, lambda e: e.tensor_reduce(out.ap, in_.ap, axis, op), [in_], [out])

    def recip(self, out, in_):
        return self.op("dve", lambda e: e.reciprocal(out.ap, in_.ap), [in_], [out])


def dram_in(nc, name, shape, dt=F32):
    return V(nc.dram_tensor(name, list(shape), dt, kind="ExternalInput").ap(), ("dram_" + name,))


def dram_out(nc, name, shape, dt=F32):
    return V(nc.dram_tensor(name, list(shape), dt, kind="ExternalOutput").ap(), ("dram_" + name,))


def dv(v, idx_fn):
    return V(idx_fn(v.ap), v.keys)


D = 2048
DIN = 4032
NT = 9
TOK = NT * 128
NCORES = 8
CTX = {}
CW = 504


def barrier(S):
    engs = ["pe", "dve", "act", "pool"]
    evs = [Ev(e, S.cnt[e], {}, e) for e in engs if S.cnt[e]]
    for e in engs + ["sp"]:
        for ev in evs:
            if ev.prod != e:
                S._wait(e, ev)
    for s in range(S.NDMA):
        if S.dcnt[s]:
            ev = Ev(("dma", s), S.dcnt[s], {}, "sp")
            for e in engs + ["sp"]:
                S._wait(e, ev)


def rr(i, engs=("dve", "act", "pool")):
    return engs[i % len(engs)]


def build_A():
    nc = CTX["nc"]
    io = CTX["io"]
    xin = io["xin"]
    cT = io["cT"]
    mod_w = io["mod_w"]
    mod_b = io["mod_b"]
    g1 = io["g1"]
    w_in = io["w_in"]
    ident = io["ident"]
    P = io["P"]
    mod = io["mod"]
    with ExitStack() as st:
        S = CTX["S"]
        S.stack = st
        idt = S.sb([128, 128], name="idt")
        S.dma("sp", idt[:], ident)
        idb = S.sb([128, 128], BF16, name="idb")
        S.copy("dve", idb[:], idt[:])
        with ExitStack() as st1:
          if CTX.get("do_mod", True):
              scT = S.sb([128, 32], name="scT", stack=st1)
              craw = S.sb([128, 32], name="craw", stack=st1)
              S.dma("sp", craw[:], cT)
              S.act(scT[:], craw[:], AF.Silu)
              mb = S.sb([2, 6 * D], name="mb", stack=st1)
              S.dma("act", mb[0:1, :], mod_b)
              S.dma("act", mb[1:2, :], mod_b)
              NB = 3
              wbuf = [S.sb([128, 16, 512], name="modw%d" % i, stack=st1) for i in range(NB)]
              pm = [S.ps([2, 512], name="pm%d" % i, stack=st1) for i in range(2)]
              mo = [S.sb([2, 512], name="mo%d" % i, stack=st1) for i in range(2)]
              for j in range(24):
                  wb = wbuf[j % NB]
                  src = dv(mod_w, lambda a: a[:, j * 512:(j + 1) * 512].rearrange("(c p) n -> p c n", p=128))
                  for h in range(2):
                      S.dma(("sp", "pool")[h], wb.k(h, (slice(None), slice(h * 8, h * 8 + 8), slice(None))),
                            dv(src, lambda a: a[:, h * 8:h * 8 + 8, :]))
                  for c in range(16):
                      S.mm(pm[j % 2][:], scT[:, 2 * c:2 * c + 2], wb.k(c // 8, (slice(None), c, slice(None))),
                           start=(c == 0), stop=(c == 15))
                  S.tt("dve", mo[j % 2][:], pm[j % 2][:], mb[:, j * 512:(j + 1) * 512], ALU.add)
                  S.dma("act", dv(mod, lambda a: a[:, j * 512:(j + 1) * 512]), mo[j % 2][:], is_output=True)
              barrier(S)
        g1b = S.sb([128, D], name="g1b")
        S.dma("sp", g1b[:], dv(g1, lambda a: a.partition_broadcast(128)))
        gs = []
        sh = []
        for r in range(2):
            sc_b = S.sb([128, D], name="scb%d" % r)
            sh_b = S.sb([128, D], name="shb%d" % r)
            S.dma("sp", sh_b[:], dv(mod, lambda a: a[r:r + 1, 0:D].partition_broadcast(128)))
            S.dma("pool", sc_b[:], dv(mod, lambda a: a[r:r + 1, D:2 * D].partition_broadcast(128)))
            S.stt("dve", sc_b[:], sc_b[:], 1.0, g1b[:], ALU.add, ALU.mult)
            gs.append(sc_b)
            sh.append(sh_b)
        hT = S.sb([128, NT, 16, 128], BF16, name="hT")
        xt = [S.sb([128, D], name="xt%d" % i) for i in range(2)]
        tmp = S.sb([128, D], name="tmpA")
        hb = S.sb([128, D], BF16, name="hb")
        stat = S.sb([128, NT, 4], name="statA")
        ptr = [S.ps([128, 4, 128], BF16, name="ptr%d" % i) for i in range(2)]
        for i in range(NT):
            r = 1 if i == 0 else 0
            x_t = xt[i % 2]
            S.dma(("sp", "pool")[i % 2], x_t[:], dv(xin, lambda a: a[i * 128:(i + 1) * 128, :]))
            S.act(tmp[:], x_t[:], AF.Square, accum=stat[:, i, 0:1])
            S.ts("dve", stat[:, i, 1:2], stat[:, i, 0:1], 1.0 / D, 1e-6, ALU.mult, ALU.add)
            S.act(stat[:, i, 2:3], stat[:, i, 1:2], AF.Sqrt)
            S.recip(stat[:, i, 3:4], stat[:, i, 2:3])
            S.stt("dve", tmp[:], x_t[:], stat[:, i, 3:4], gs[r][:], ALU.mult, ALU.mult)
            S.tt("pool", hb[:], tmp[:], sh[r][:], ALU.add)
            for q in range(4):
                pt = ptr[q % 2]
                for c4 in range(4):
                    c = q * 4 + c4
                    S.tr(pt[:, c4, :], hb[:, c * 128:(c + 1) * 128], idb[:])
                S.copy(("dve", "act")[q % 2], hT[:, i, q * 4:q * 4 + 4, :], pt[:])
        NS = 4
        wst = [S.sb([128, 4, CW], name="wst%d" % i) for i in range(NS)]
        wbf = [S.sb([128, 16, CW], BF16, name="wbf%d" % i) for i in range(2)]
        po = [S.ps([128, CW], name="poA%d" % i) for i in range(3)]
        ot = [S.sb([128, CW], name="otA%d" % i) for i in range(3)]
        n = 0
        k = 0
        for j in range(DIN // CW):
            wb = wbf[j % 2]
            for q in range(4):
                ws = wst[k % NS]
                S.dma(("sp", "pool")[k % 2], ws[:],
                      dv(w_in, lambda a: a[q * 512:(q + 1) * 512, j * CW:(j + 1) * CW].rearrange("(c p) n -> p c n", p=128)))
                S.copy(rr(k), wb.k(q, (slice(None), slice(q * 4, q * 4 + 4), slice(None))), ws[:])
                k += 1
            for i in range(NT):
                p_ = po[n % 3]
                o_ = ot[n % 3]
                for c in range(16):
                    S.mm(p_[:], hT[:, i, c, :], wb.k(c // 4, (slice(None), c, slice(None))), start=(c == 0), stop=(c == 15))
                S.copy(("dve", "act")[n % 2], o_[:], p_[:])
                S.dma("act" if n % 2 else "sp", dv(P, lambda a: a[i * 128:(i + 1) * 128, j * CW:(j + 1) * CW]), o_[:], is_output=True)
                n += 1
        barrier(S)
        print("A ninstr", S.ninstr)
    return nc


def core_tokens(x, ctx, core):
    b, h = core // 2, core % 2
    return np.concatenate([ctx[b, h * 128:(h + 1) * 128], x[b, h * 1024:(h + 1) * 1024]], axis=0)


def cT_layout(c_b, c_ctx):
    cv = np.stack([c_b, c_ctx], axis=0)
    return np.ascontiguousarray(cv.reshape(2, 16, 128).transpose(2, 1, 0).reshape(128, 32))


_NC = {}


def get_nc(name, fn):
    if name not in _NC:
        _NC[name] = fn()
    return _NC[name]


def run_A(x, ctx, c, c_ctx, mod_w_l, mod_b_l, g1_l, w_in_l):
    nc = get_nc("A", build_A)
    ident = np.eye(128, dtype=np.float32)
    maps = []
    for core in range(NCORES):
        maps.append(dict(xin=np.ascontiguousarray(core_tokens(x, ctx, core)), cT=cT_layout(c[core // 2], c_ctx),
                         mod_w=mod_w_l, mod_b=mod_b_l.reshape(1, -1), g1=g1_l.reshape(1, -1), w_in=w_in_l, ident=ident))
    res = run_bass_kernel_spmd(nc, maps, core_ids=list(range(NCORES)))
    return [r["P"] for r in res.results], [r["mod"] for r in res.results]


NTB = 18
TB = NTB * 128
PA_W = 1472


def rstd_of(S, out, ss, width, eps, tmp):
    S.ts("dve", tmp[0], ss, 1.0 / width, eps, ALU.mult, ALU.add)
    S.act(tmp[1], tmp[0], AF.Sqrt)
    S.recip(out, tmp[1])


def rope_tm(S, eng, out, x, cos, sin, nh, qd, t1, t2):
    def v5(v):
        return v.ap.rearrange("p (h a two j) -> p h a two j", h=nh, a=2, two=2)
    xv = v5(x)
    ov = v5(out)
    x1 = V(xv[:, :, :, 0, :], x.keys)
    x2 = V(xv[:, :, :, 1, :], x.keys)
    o1 = V(ov[:, :, :, 0, :], out.keys)
    o2 = V(ov[:, :, :, 1, :], out.keys)
    def bc(v):
        a = v.ap.rearrange("p (a j) -> p a j", a=2).unsqueeze(1).to_broadcast([128, nh, 2, qd])
        return V(a, v.keys)
    c = bc(cos)
    s = bc(sin)
    def sh(v):
        return V(v.ap.rearrange("p (h a j) -> p h a j", h=nh, a=2), v.keys)
    a1 = sh(t1)
    a2 = sh(t2)
    S.tt(eng, a1, x1, c, ALU.mult)
    S.tt(eng, a2, x2, s, ALU.mult)
    S.tt(eng, o1, a1, a2, ALU.subtract)
    S.tt(eng, a1, x1, s, ALU.mult)
    S.tt(eng, a2, x2, c, ALU.mult)
    S.tt(eng, o2, a1, a2, ALU.add)


def build_Battn():
    nc = CTX["nc"]
    io = CTX["io"]
    Pa = io["Pa"]
    gq = io["gq"]
    gk = io["gk"]
    gmq = io["gmq"]
    gmkv = io["gmkv"]
    wuq = io["wuq"]
    wukv = io["wukv"]
    cosG = io["cosG"]
    sinG = io["sinG"]
    cosM = io["cosM"]
    sinM = io["sinM"]
    ident = io["ident"]
    Oa = io["Oa"]
    SC_G = 128 ** -0.5
    SC_M = 192 ** -0.5
    with ExitStack() as st:
        S = CTX["S"]
        S.stack = st
        idt = S.sb([128, 128], name="idt")
        S.dma("sp", idt[:], ident)
        idb = S.sb([128, 128], BF16, name="idb")
        S.copy("dve", idb[:], idt[:])
        def bload(src, n, name, q="sp"):
            t = S.sb([128, n], name=name)
            S.dma(q, t[:], dv(src, lambda a: a.partition_broadcast(128)))
            return t
        gq_b = bload(gq, 128, "gq_b")
        gk_b = bload(gk, 128, "gk_b", "act")
        gmq_b = bload(gmq, 384, "gmq_b", "pool")
        gmkv_b = bload(gmkv, 256, "gmkv_b")
        wuq_f = S.sb([128, 3, 384], name="wuq_f")
        S.dma("sp", wuq_f[:], dv(wuq, lambda a: a.rearrange("(c p) n -> p c n", p=128)))
        wuq_b = S.sb([128, 3, 384], BF16, name="wuq_b")
        S.copy("pool", wuq_b[:], wuq_f[:])
        wukv_f = S.sb([128, 2, 512], name="wukv_f")
        S.dma("act", wukv_f[:], dv(wukv, lambda a: a.rearrange("(c p) n -> p c n", p=128)))
        wukv_b = S.sb([128, 2, 512], BF16, name="wukv_b")
        S.copy("pool", wukv_b[:], wukv_f[:])
        KT = S.sb([128, TB], BF16, name="KT")
        Vg = S.sb([128, NTB, 129], BF16, name="Vg")
        KnT = S.sb([128, 2, TB], BF16, name="KnT")
        krT = S.sb([64, TB], BF16, name="krT")
        Vm = S.sb([128, NTB, 2, 129], BF16, name="Vm")
        S.memset("pool", Vg[:, :, 128:129], 1.0)
        S.memset("pool", Vm[:, :, :, 128:129], 1.0)
        kvt = [S.sb([128, 576], name="kvt%d" % i) for i in range(2)]
        cs = [S.sb([128, 192], name="cs%d" % i) for i in range(2)]
        junk = S.sb([128, 512], name="junk")
        stat = [S.sb([128, 16], name="stat%d" % i) for i in range(2)]
        f1 = S.sb([128, 512], name="f1")
        f2 = S.sb([128, 512], name="f2")
        r1 = S.sb([128, 256], name="r1")
        r2 = S.sb([128, 256], name="r2")
        b1 = S.sb([128, 512], BF16, name="b1")
        b2 = S.sb([128, 384], BF16, name="b2")
        b3 = S.sb([128, 128], BF16, name="b3")
        ckT = S.sb([128, 2, 128], BF16, name="ckT")
        ptr = [S.ps([128, 4, 128], BF16, name="ptr%d" % i) for i in range(1)]
        pmm = [S.ps([128, 512], name="pmm%d" % i) for i in range(1)]
        npm = [0]
        def next_pmm():
            npm[0] += 1
            return pmm[npm[0] % 1]

        def load_cs(i, buf):
            p0 = (i - 2) * 128
            S.dma("sp", buf[:, 0:64], dv(cosG, lambda a: a[p0:p0 + 128, :]))
            S.dma("act", buf[:, 64:128], dv(sinG, lambda a: a[p0:p0 + 128, :]))
            S.dma("sp", buf[:, 128:160], dv(cosM, lambda a: a[p0:p0 + 128, :]))
            S.dma("act", buf[:, 160:192], dv(sinM, lambda a: a[p0:p0 + 128, :]))

        for i in range(NTB):
            lat = i >= 2
            kv = kvt[i % 2]
            st_ = stat[i % 2]
            csb = cs[i % 2]
            r0 = i * 128
            S.dma("sp", kv[:, 0:256], dv(Pa, lambda a: a[r0:r0 + 128, 512:768]))
            S.dma("pool", kv[:, 256:576], dv(Pa, lambda a: a[r0:r0 + 128, 1152:1472]))
            if lat:
                load_cs(i, csb)
            S.act(junk[:, 0:128], kv[:, 0:128], AF.Square, accum=st_[:, 0:1])
            rstd_of(S, st_[:, 1:2], st_[:, 0:1], 128, 1e-6, (st_[:, 2:3], st_[:, 3:4]))
            S.stt("dve", f1[:, 0:128], kv[:, 0:128], st_[:, 1:2], gk_b[:], ALU.mult, ALU.mult)
            if lat:
                rope_tm(S, "pool", b3[:], f1[:, 0:128], csb[:, 0:64], csb[:, 64:128], 1, 32, r1[:, 0:64], r2[:, 0:64])
            else:
                S.copy("pool", b3[:], f1[:, 0:128])
            S.tr(ptr[0][:, 0, :], b3[:], idb[:])
            S.copy("dve", KT[:, r0:r0 + 128], ptr[0][:, 0, :])
            S.copy("pool", Vg[:, i, 0:128], kv[:, 128:256])
            S.act(junk[:, 0:256], kv[:, 256:512], AF.Square, accum=st_[:, 4:5])
            rstd_of(S, st_[:, 5:6], st_[:, 4:5], 256, 1e-6, (st_[:, 6:7], st_[:, 7:8]))
            S.stt("dve", b1[:, 0:256], kv[:, 256:512], st_[:, 5:6], gmkv_b[:], ALU.mult, ALU.mult)
            for c in range(2):
                S.tr(ptr[0][:, 1 + c, :], b1[:, c * 128:(c + 1) * 128], idb[:])
            S.copy("dve", ckT[:], ptr[0][:, 1:3, :])
            pk = next_pmm()
            for hh in range(2):
                for c in range(2):
                    S.mm(pk[:, hh * 128:(hh + 1) * 128], wukv_b[:, c, hh * 256:hh * 256 + 128], ckT[:, c, :],
                         start=(c == 0), stop=(c == 1))
            S.copy("act", KnT[:, :, r0:r0 + 128], vw(pk[:, 0:256], lambda a: a.rearrange("p (h t) -> p h t", h=2)))
            pv = next_pmm()
            for hh in range(2):
                for c in range(2):
                    S.mm(pv[:, hh * 128:(hh + 1) * 128], ckT[:, c, :], wukv_b[:, c, hh * 256 + 128:hh * 256 + 256],
                         start=(c == 0), stop=(c == 1))
            S.copy("dve", Vm[:, i, :, 0:128], vw(pv[:, 0:256], lambda a: a.rearrange("p (h d) -> p h d", h=2)))
            if lat:
                rope_tm(S, "pool", b2[:, 0:64], kv[:, 512:576], csb[:, 128:160], csb[:, 160:192], 1, 16, r1[:, 64:96], r2[:, 64:96])
            else:
                S.copy("pool", b2[:, 0:64], kv[:, 512:576])
            S.tr(ptr[0][0:64, 3, :], b2[:, 0:64], idb[:])
            S.copy("dve", krT[:, r0:r0 + 128], ptr[0][0:64, 3, :])

        QT2 = [S.sb([128, 4, 512], BF16, name="QT%d" % i) for i in range(2)]
        cqT2 = [S.sb([128, 3, 512], BF16, name="cqT%d" % i) for i in range(2)]
        QnT2 = [S.sb([128, 2, 512], BF16, name="QnT%d" % i) for i in range(2)]
        QrT2 = [S.sb([64, 2, 512], BF16, name="QrT%d" % i) for i in range(2)]
        qt = [S.sb([128, 896], name="qt%d" % i) for i in range(2)]
        Et = [S.sb([128, 512], BF16, name="Et%d" % i) for i in range(3)]
        psT = [S.ps([128, 512], name="psT%d" % i) for i in range(2)]
        pacc = [S.ps([128, 512], name="pacc%d" % i) for i in range(4)]
        otile = [S.sb([128, 768], name="otile%d" % i) for i in range(4)]
        rs = S.sb([128, 8], name="rs")
        nE = [0]
        nrs = [0]

        def attend(nq, ktiles, score_ops, vfn, scale, ocol):
            ntq = nq // 128
            nk = len(ktiles)
            def scores(kt):
                ps = psT[nE[0] % 2]
                E = Et[nE[0] % 3]
                nE[0] += 1
                ops = score_ops(kt)
                for oi, (l_, r_) in enumerate(ops):
                    S.mm(ps[:, 0:nq], l_, r_, start=(oi == 0), stop=(oi == len(ops) - 1))
                return ps, E
            cur = scores(ktiles[0])
            for ki, kt in enumerate(ktiles):
                ps, E = cur
                S.act(E[:, 0:nq], ps[:, 0:nq], AF.Exp, scale=scale)
                if ki + 1 < nk:
                    cur = scores(ktiles[ki + 1])
                vv = vfn(kt)
                for j in range(ntq):
                    S.mm(pacc[j][:, 0:129], E[:, j * 128:(j + 1) * 128], vv, start=(ki == 0), stop=(ki == nk - 1))
                yield
            for j in range(ntq):
                c = nrs[0] % 8
                nrs[0] += 1
                S.recip(rs[:, c:c + 1], pacc[j][:, 128:129])
                S.ts("dve", otile[j][:, ocol:ocol + 128], pacc[j][:, 0:128], rs[:, c:c + 1], None, ALU.mult)

        if CTX.get("own_only", False):
            groups = [([0], [0, 1])] + [(list(range(2 + 4 * g, 6 + 4 * g)), list(range(NTB))) for g in range(2)]
        else:
            groups = [(list(range(0, 2)), list(range(0, 2)))] + [(list(range(2 + 4 * g, 6 + 4 * g)), list(range(NTB))) for g in range(4)]
        nq_c = [0]

        def prep(gi):
            qtiles, ktiles = groups[gi]
            QT, cqT, QnT, QrT = QT2[gi % 2], cqT2[gi % 2], QnT2[gi % 2], QrT2[gi % 2]
            nq = len(qtiles) * 128
            for j, i in enumerate(qtiles):
                lat = i >= 2
                q_ = qt[nq_c[0] % 2]
                st_ = stat[nq_c[0] % 2]
                csb = cs[nq_c[0] % 2]
                nq_c[0] += 1
                r0 = i * 128
                S.dma("sp", q_[:, 0:512], dv(Pa, lambda a: a[r0:r0 + 128, 0:512]))
                S.dma("pool", q_[:, 512:896], dv(Pa, lambda a: a[r0:r0 + 128, 768:1152]))
                if lat:
                    load_cs(i, csb)
                S.tt("pool", f1[:], q_[:, 0:512], q_[:, 0:512], ALU.mult)
                S.reduce("dve", st_[:, 8:12], V(f1.t[:].rearrange("p (h d) -> p h d", h=4), (f1.name,)), ALU.add)
                S.ts("dve", st_[:, 12:16], st_[:, 8:12], 1.0 / 128, 1e-6, ALU.mult, ALU.add)
                S.act(st_[:, 8:12], st_[:, 12:16], AF.Sqrt)
                S.recip(st_[:, 12:16], st_[:, 8:12])
                S.tt("dve", V(f2.t[:].rearrange("p (h d) -> p h d", h=4), (f2.name,)),
                     V(q_.t[:, 0:512].rearrange("p (h d) -> p h d", h=4), (q_.name,)),
                     V(st_.t[:, 12:16].unsqueeze(2).to_broadcast([128, 4, 128]), (st_.name,)), ALU.mult)
                if lat:
                    S.tt("pool", V(f1.t[:].rearrange("p (h d) -> p h d", h=4), (f1.name,)),
                         V(f2.t[:].rearrange("p (h d) -> p h d", h=4), (f2.name,)),
                         V(gq_b.t[:].unsqueeze(1).to_broadcast([128, 4, 128]), (gq_b.name,)), ALU.mult)
                    rope_tm(S, "pool", b1[:], f1[:], csb[:, 0:64], csb[:, 64:128], 4, 32, r1[:], r2[:])
                else:
                    S.tt("pool", V(b1.t[:].rearrange("p (h d) -> p h d", h=4), (b1.name,)),
                         V(f2.t[:].rearrange("p (h d) -> p h d", h=4), (f2.name,)),
                         V(gq_b.t[:].unsqueeze(1).to_broadcast([128, 4, 128]), (gq_b.name,)), ALU.mult)
                for h in range(4):
                    S.tr(ptr[0][:, h, :], b1[:, h * 128:(h + 1) * 128], idb[:])
                S.copy("dve", QT[:, :, j * 128:(j + 1) * 128], ptr[0][:])
                S.act(junk[:, 0:384], q_[:, 512:896], AF.Square, accum=st_[:, 4:5])
                rstd_of(S, st_[:, 5:6], st_[:, 4:5], 384, 1e-6, (st_[:, 6:7], st_[:, 7:8]))
                S.stt("dve", b2[:], q_[:, 512:896], st_[:, 5:6], gmq_b[:], ALU.mult, ALU.mult)
                for c in range(3):
                    S.tr(ptr[0][:, c, :], b2[:, c * 128:(c + 1) * 128], idb[:])
                S.copy("dve", cqT[:, :, j * 128:(j + 1) * 128], ptr[0][:, 0:3, :])
                pq = next_pmm()
                for hh in range(2):
                    for c in range(3):
                        S.mm(pq[:, hh * 64:(hh + 1) * 64], cqT[:, c, j * 128:(j + 1) * 128],
                             wuq_b[:, c, hh * 192 + 128:hh * 192 + 192], start=(c == 0), stop=(c == 2))
                if lat:
                    S.copy("act", f2[:, 0:128], pq[:, 0:128])
                    rope_tm(S, "pool", b3[:], f2[:, 0:128], csb[:, 128:160], csb[:, 160:192], 2, 16, r1[:, 0:64], r2[:, 0:64])
                else:
                    S.copy("act", b3[:], pq[:, 0:128])
                for hh in range(2):
                    S.tr(ptr[0][0:64, hh, :], b3[:, hh * 64:(hh + 1) * 64], idb[:])
                S.copy("dve", QrT[:, :, j * 128:(j + 1) * 128], ptr[0][0:64, 0:2, :])
            for hh in range(2):
                pq = next_pmm()
                for c in range(3):
                    S.mm(pq[:, 0:nq], wuq_b[:, c, hh * 192:hh * 192 + 128], cqT[:, c, 0:nq], start=(c == 0), stop=(c == 2))
                S.copy("act", QnT[:, hh, 0:nq], pq[:, 0:nq])

        def attn_gen(gi):
            qtiles, ktiles = groups[gi]
            nq = len(qtiles) * 128
            QT, cqT, QnT, QrT = QT2[gi % 2], cqT2[gi % 2], QnT2[gi % 2], QrT2[gi % 2]
            for h in range(4):
                yield from attend(nq, ktiles, lambda kt: [(KT[:, kt * 128:(kt + 1) * 128], QT[:, h, 0:nq])],
                       lambda kt: Vg[:, kt, :], SC_G, h * 128)
            for hh in range(2):
                yield from attend(nq, ktiles, lambda kt: [(KnT[:, hh, kt * 128:(kt + 1) * 128], QnT[:, hh, 0:nq]),
                                               (krT[:, kt * 128:(kt + 1) * 128], QrT[:, hh, 0:nq])],
                       lambda kt: Vm[:, kt, hh, :], SC_M, 512 + hh * 128)
            for j, i in enumerate(qtiles):
                S.dma(("sp", "act")[j % 2], dv(Oa, lambda a: a[i * 128:(i + 1) * 128, :]), otile[j][:], is_output=True)

        prep(0)
        for gi in range(len(groups)):
            S.bg = attn_gen(gi)
            S.bgk = 2
            if gi + 1 < len(groups):
                prep(gi + 1)
            S.drain_bg()
        S.bgk = 6
        barrier(S)
        print("Battn ninstr", S.ninstr)
    return nc


def rope_tables():
    t = np.arange(2048)
    row = (t // 64).astype(np.float32)
    col = (t % 64).astype(np.float32)
    def tab(q):
        inv = (10000.0 ** (-np.arange(q, dtype=np.float32) / q)).astype(np.float32)
        ar = row[:, None] * inv[None, :]
        ac = col[:, None] * inv[None, :]
        return (np.concatenate([np.cos(ar), np.cos(ac)], 1).astype(np.float32),
                np.concatenate([np.sin(ar), np.sin(ac)], 1).astype(np.float32))
    cG, sG = tab(32)
    cM, sM = tab(16)
    return cG, sG, cM, sM


def attn_cols(g):
    q0 = 1792
    cols = list(range(q0 + 512 * g, q0 + 512 * g + 512))
    cols += list(range(q0 + 1024 + 128 * g, q0 + 1024 + 128 * g + 128))
    cols += list(range(q0 + 1280 + 128 * g, q0 + 1280 + 128 * g + 128))
    m0 = 1792 + 1536
    cols += list(range(m0, m0 + 384 + 256 + 64))
    return np.array(cols)


def run_Battn(Pfull, q_norm_g, k_norm_g, mla_q_norm_g, mla_w_uq, mla_kv_norm_g, mla_w_ukv):
    nc = get_nc("Battn", build_Battn)
    ident = np.eye(128, dtype=np.float32)
    cG, sG, cM, sM = rope_tables()
    maps = []
    for core in range(NCORES):
        b, g = core // 2, core % 2
        wuq = np.concatenate([mla_w_uq[:, (2 * g + hh) * 192:(2 * g + hh + 1) * 192] for hh in range(2)], 1)
        wukv = np.concatenate([mla_w_ukv[:, (2 * g + hh) * 256:(2 * g + hh + 1) * 256] for hh in range(2)], 1)
        maps.append(dict(Pa=np.ascontiguousarray(Pfull[b][:, attn_cols(g)]), gq=q_norm_g.reshape(1, -1), gk=k_norm_g.reshape(1, -1),
                         gmq=mla_q_norm_g.reshape(1, -1), gmkv=mla_kv_norm_g.reshape(1, -1),
                         wuq=np.ascontiguousarray(wuq), wukv=np.ascontiguousarray(wukv),
                         cosG=cG, sinG=sG, cosM=cM, sinM=sM, ident=ident))
    res = run_bass_kernel_spmd(nc, maps, core_ids=list(range(NCORES)))
    return [r["Oa"] for r in res.results]


DECAY_SCALE = float(np.exp(-0.5))
GN_EPS = 64e-5


def vw(v, fn):
    return V(fn(v.ap), v.keys)


def h4(v, n=64):
    return vw(v, lambda a: a.rearrange("p (h d) -> p h d", h=4))


DBG = {"units": 2 * NTB, "stage": 99}


def build_Brwkv():
    nc = CTX["nc"]
    io = CTX["io"]
    Pr = io["Pr"]
    mup = io["mup"]
    mun = io["mun"]
    w0a0 = io["w0a0"]
    wup = io["wup"]
    aup = io["aup"]
    gup = io["gup"]
    vecs = io["vecs"]
    consts = io["consts"]
    Or = io["Or"]
    with ExitStack() as st:
        S = CTX["S"]
        S.stack = st
        cst = S.sb([128, 6, 128], name="cst")
        S.dma("sp", cst[:], dv(consts, lambda a: a.rearrange("p (c n) -> p c n", c=6)))
        ident = cst[:, 5, :]
        same = cst[:, 4, :]
        blockind = vw(cst[:, 4, :], lambda a: a[:, 0:128:64])
        def bload(src, n, name, q="sp"):
            t = S.sb([128, n], name=name)
            S.dma(q, t[:], dv(src, lambda a: a.partition_broadcast(128)))
            return t
        mup_b = bload(mup, 1024, "mup_b")
        mun_b = bload(mun, 1024, "mun_b", "act")
        w0a0_b = bload(w0a0, 1024, "w0a0_b", "pool")
        vec_b = bload(vecs, 1280, "vec_b")
        kk_b = vec_b[:, 0:256]
        ka_b = vec_b[:, 256:512]
        rk_b = vec_b[:, 512:768]
        gng_b = vec_b[:, 768:1024]
        gnb_b = vec_b[:, 1024:1280]
        oka_b = S.sb([128, 256], name="oka_b")
        S.ts("dve", oka_b[:], ka_b, -1.0, 1.0, ALU.mult, ALU.add)
        wup_s = S.sb([64, 512], name="wup_s")
        aup_s = S.sb([64, 512], name="aup_s")
        gup_s = S.sb([128, 256], name="gup_s")
        S.dma("sp", wup_s[:], wup)
        S.dma("act", aup_s[:], aup)
        S.dma("pool", gup_s[:], gup)
        Yd = [S.sb([128, NTB, 256], name="Yd%d" % d) for d in range(2)]
        vS = S.sb([128, NTB, 256], name="vS")
        gS = S.sb([128, NTB, 256], name="gS")
        bon = S.sb([128, NTB, 8], name="bon")
        A = [S.sb([64, 4, 64], name="A%d" % d) for d in range(2)]
        for d in range(2):
            S.memset("pool", A[d][:], 0.0)
        pt = S.sb([128, 1024], name="pt")
        prv = S.sb([128, 1024], name="prv")
        nx = S.sb([128, 1024], name="nx")
        zt = S.sb([128, 1024], name="zt")
        def T256(name):
            return S.sb([128, 256], name=name)
        khat, tA, tB, lw, kt, bb, csS, E1, E2, E3, E4, kap, bet, gam, rho = [T256(n) for n in
            ("khat", "tA", "tB", "lw", "kt", "bb", "csS", "E1", "E2", "E3", "E4", "kap", "bet", "gam", "rho")]
        s1 = T256("s1")
        sg = S.sb([128, 512], name="sg")
        stt_ = S.sb([128, 16], name="stt_")
        lT = S.sb([128, 3, 128], name="lT")
        kapT = S.sb([64, 4, 128], name="kapT")
        betT = S.sb([64, 4, 128], name="betT")
        gamT = S.sb([64, 4, 128], name="gamT")
        def T4(name):
            return S.sb([128, 4, 128], name=name)
        Zb = [T4("Zb%d" % i) for i in range(2)]
        ZTb = [T4("ZTb%d" % i) for i in range(2)]
        G = T4("G")
        LgT = T4("LgT")
        MgT = T4("MgT")
        X1 = S.sb([128, 4, 64], name="X1")
        NSQ = 2
        sq = []
        for i in range(NSQ):
            sq.append(dict(
                W1T=S.sb([64, 4, 128], name="W1T%d" % i), W2=S.sb([128, 4, 64], name="W2%d" % i),
                rhoT=S.sb([64, 4, 128], name="rhoT%d" % i), MbT=T4("MbT%d" % i), Y0=S.sb([128, 4, 64], name="Y0%d" % i),
                betp=T256("betp%d" % i), gamp=T256("gamp%d" % i), vq=T256("vq%d" % i),
                PC=S.sb([64, 4, 2], name="PC%d" % i), U=[S.sb([128, 4, 64], name="U%d_%d" % (i, b_)) for b_ in range(2)],
                gampm=S.sb([128, 2, 256], name="gampm%d" % i)))
            for b_ in range(2):
                S.memset("pool", sq[i]["U"][b_][:], 0.0)
        pm = [S.ps([128, 512], name="pm%d" % i) for i in range(3)]
        pbig = [S.ps([128, 4, 128], name="pbig%d" % i) for i in range(2)]
        pU = S.ps([128, 4, 64], name="pU")
        pY = S.ps([128, 4, 64], name="pY")
        pA = S.ps([64, 4, 64], name="pA")
        cnt = {"pm": 0, "pbig": 0}
        def npm():
            cnt["pm"] += 1
            return pm[cnt["pm"] % 3]
        def npb():
            cnt["pbig"] += 1
            return pbig[cnt["pbig"] % 2]
        def bc4(v):
            return vw(v, lambda a: a.unsqueeze(1).to_broadcast([128, 4, 128]))
        def bh(v, n=64):
            return vw(v, lambda a: a.unsqueeze(2).to_broadcast([a.shape[0], 4, n]))

        def unit(i, d, Q, first):
            r0 = i * 128
            S.dma("sp", pt[:], dv(Pr, lambda a: a[r0:r0 + 128, :]))
            if i in (0, 2):
                S.memset("pool", prv[:], 0.0)
                S.dma("pool", prv[1:128, :], dv(Pr, lambda a: a[r0:r0 + 127, :]))
            else:
                S.dma("pool", prv[:], dv(Pr, lambda a: a[r0 - 1:r0 + 127, :]))
            if i in (1, NTB - 1):
                S.memset("pool", nx[:], 0.0)
                S.dma("act", nx[0:127, :], dv(Pr, lambda a: a[r0 + 1:r0 + 128, :]))
            else:
                S.dma("act", nx[:], dv(Pr, lambda a: a[r0 + 1:r0 + 129, :]))
            S.tt("pool", prv[:], prv[:], pt[:], ALU.subtract)
            S.tt("pool", prv[:], prv[:], mup_b[:], ALU.mult)
            S.tt("dve", nx[:], nx[:], pt[:], ALU.subtract)
            S.tt("dve", nx[:], nx[:], mun_b[:], ALU.mult)
            S.tt("pool", zt[:], pt[:], prv[:], ALU.add)
            S.tt("dve", zt[:], zt[:], nx[:], ALU.add)
            r_ = zt[:, 0:256]
            k_ = zt[:, 256:512]
            v_ = zt[:, 512:768]
            S.tt("pool", khat[:], k_, kk_b, ALU.mult)
            S.tt("pool", tA[:], khat[:], khat[:], ALU.mult)
            S.reduce("dve", stt_[:, 0:4], h4(tA[:]), ALU.add)
            S.ts("dve", stt_[:, 4:8], stt_[:, 0:4], 1e-12, None, ALU.add)
            S.act(stt_[:, 8:12], stt_[:, 4:8], AF.Sqrt)
            S.recip(stt_[:, 12:16], stt_[:, 8:12])
            S.tt("dve", h4(khat[:]), h4(khat[:]), bh(stt_[:, 12:16]), ALU.mult)
            S.act(s1[:, 0:64], zt[:, 768:832], AF.Tanh)
            if first:
                S.act(s1[:, 128:256], zt[:, 896:1024], AF.Sigmoid)
            p_ = npm()
            pv3 = vw(p_[:, 0:384], lambda a: a.rearrange("p (c n) -> p c n", c=3))
            S.tr(vw(pv3, lambda a: a[0:64, 0, :]), s1[:, 0:64], ident)
            S.tr(vw(pv3, lambda a: a[0:64, 1, :]), zt[:, 832:896], ident)
            if first:
                S.tr(vw(pv3, lambda a: a[:, 2, :]), s1[:, 128:256], ident)
                S.copy("act", lT[:, 2, :], vw(pv3, lambda a: a[:, 2, :]))
            S.copy("dve", lT[0:64, 0:2, :], vw(pv3, lambda a: a[0:64, 0:2, :]))
            p_ = npm()
            S.mm(p_[:, 0:256], lT[0:64, 0, :], wup_s[:, d * 256:(d + 1) * 256])
            S.mm(p_[:, 256:512], lT[0:64, 1, :], aup_s[:, d * 256:(d + 1) * 256])
            S.tt("dve", sg[:], p_[:], w0a0_b[:, d * 512:(d + 1) * 512], ALU.add)
            S.act(sg[:], sg[:], AF.Sigmoid)
            S.ts("pool", lw[:], sg[:, 0:256], -DECAY_SCALE, None, ALU.mult)
            a_ = sg[:, 256:512]
            S.tt("pool", tA[:], a_, ka_b, ALU.mult)
            S.tt("pool", tA[:], tA[:], oka_b[:], ALU.add)
            S.tt("pool", kt[:], k_, tA[:], ALU.mult)
            S.tt("dve", bb[:], a_, khat[:], ALU.mult)
            if first:
                p_ = npm()
                S.mm(p_[:, 0:256], lT[:, 2, :], gup_s[:])
                S.copy("act", gS[:, i, :], p_[:, 0:256])
                S.copy("pool", vS[:, i, :], v_)
            S.copy("pool", Q["vq"][:], v_)
            S.tt("pool", tB[:], r_, kt[:], ALU.mult)
            S.tt("pool", tB[:], tB[:], rk_b, ALU.mult)
            S.reduce("dve", bon[:, i, d * 4:(d + 1) * 4], h4(tB[:]), ALU.add)
            if DBG["stage"] < 1:
                return
            p_ = npm()
            S.mm(p_[:, 0:256], cst[:, 2 + d, :], lw[:])
            S.mm(p_[:, 256:512], same, lw[:])
            S.act(E1[:], p_[:, 0:256], AF.Exp)
            S.act(E2[:], p_[:, 0:256], AF.Exp, scale=-1.0)
            S.copy("act", csS[:], p_[:, 0:256])
            S.tt("dve", tA[:], p_[:, 0:256], lw[:], ALU.subtract)
            S.act(E3[:], tA[:], AF.Exp)
            S.tt("dve", tB[:], p_[:, 256:512], csS[:], ALU.subtract)
            S.act(E4[:], tB[:], AF.Exp)
            S.tt("pool", kap[:], khat[:], E3[:], ALU.mult)
            S.tt("dve", bet[:], bb[:], E2[:], ALU.mult)
            S.tt("pool", gam[:], kt[:], E2[:], ALU.mult)
            S.tt("dve", rho[:], r_, E1[:], ALU.mult)
            S.tt("pool", Q["betp"][:], bb[:], E4[:], ALU.mult)
            S.tt("pool", Q["gamp"][:], kt[:], E4[:], ALU.mult)
            for b_ in range(2):
                S.ts("pool", Q["gampm"][:, b_, :], Q["gamp"][:], vw(cst[:, 4, :], lambda a: a[:, 64 * b_:64 * b_ + 1]), None, ALU.mult)
            p_ = npm()
            ppc = vw(p_[0:64, 0:8], lambda a: a.rearrange("p (h c) -> p h c", h=4))
            for h in range(4):
                S.mm(vw(ppc, lambda a: a[:, h, :]), lw[:, h * 64:(h + 1) * 64], blockind)
            S.act(Q["PC"][:], ppc, AF.Exp)
            if DBG["stage"] < 2:
                return
            for (src, dst) in ((kap, kapT), (bet, betT), (gam, gamT), (rho, Q["rhoT"])):
                p_ = npm()
                p4 = vw(p_[0:64, :], lambda a: a.rearrange("p (h n) -> p h n", h=4))
                for h in range(4):
                    S.tr(vw(p4, lambda a: a[:, h, :]), src[:, h * 64:(h + 1) * 64], ident)
                S.copy("act" if dst in (kapT, gamT) else "dve", dst[:], p4)
            rhoT = Q["rhoT"]
            mS = bc4(cst[:, d, :])
            mST = bc4(cst[:, 1 - d, :])
            mI = bc4(cst[:, 2 + d, :])
            if DBG["stage"] < 3:
                return
            Z = Zb[0]
            ZT = ZTb[0]
            p_ = npb()
            for h in range(4):
                S.mm(p_[:, h, :], betT[:, h, :], kapT[:, h, :])
            S.stt("dve", Z[:], p_[:], -1.0, mS, ALU.mult, ALU.mult)
            p_ = npb()
            for h in range(4):
                S.mm(p_[:, h, :], kapT[:, h, :], betT[:, h, :])
            S.stt("dve", ZT[:], p_[:], -1.0, mST, ALU.mult, ALU.mult)
            S.tt("pool", G[:], Z[:], bc4(ident), ALU.add)
            for lev in range(5):
                Zn = Zb[(lev + 1) % 2]
                ZTn = ZTb[(lev + 1) % 2]
                if lev < 4:
                    p_ = npb()
                    for h in range(4):
                        S.mm(p_[:, h, :], ZT[:, h, :], Z[:, h, :])
                    S.copy("act", Zn[:], p_[:])
                p_ = npb()
                for h in range(4):
                    S.mm(p_[:, h, :], Z[:, h, :], ZT[:, h, :])
                S.copy("dve", ZTn[:], p_[:])
                p_ = npb()
                for h in range(4):
                    S.mm(p_[:, h, :], ZTn[:, h, :], G[:, h, :])
                S.tt("dve", G[:], G[:], p_[:], ALU.add)
                Z, ZT = Zn, ZTn
            if DBG["stage"] < 4:
                return
            p_ = npb()
            for h in range(4):
                S.mm(p_[:, h, :], gamT[:, h, :], kapT[:, h, :])
            S.tt("dve", LgT[:], p_[:], mS, ALU.mult)
            p_ = npm()
            p4 = vw(p_[:, 0:256], lambda a: a.rearrange("p (h n) -> p h n", h=4))
            for h in range(4):
                S.mm(vw(p4, lambda a: a[:, h, :]), LgT[:, h, :], zt[:, 512 + h * 64:512 + (h + 1) * 64])
            S.copy("act", X1[:], p4)
            p_ = npm()
            p4 = vw(p_[:, 0:256], lambda a: a.rearrange("p (h n) -> p h n", h=4))
            for h in range(4):
                S.mm(vw(p4, lambda a: a[:, h, :]), G[:, h, :], X1[:, h, :])
            S.copy("act", Q["W2"][:], p4)
            p_ = npm()
            p4 = vw(p_[0:64, :], lambda a: a.rearrange("p (h n) -> p h n", h=4))
            for h in range(4):
                S.mm(vw(p4, lambda a: a[:, h, :]), kap[:, h * 64:(h + 1) * 64], G[:, h, :])
            S.copy("dve", Q["W1T"][:], p4)
            p_ = npb()
            for h in range(4):
                S.mm(p_[:, h, :], betT[:, h, :], rhoT[:, h, :])
            S.tt("dve", Q["MbT"][:], p_[:], mI, ALU.mult)
            p_ = npb()
            for h in range(4):
                S.mm(p_[:, h, :], gamT[:, h, :], rhoT[:, h, :])
            S.tt("dve", MgT[:], p_[:], mI, ALU.mult)
            p_ = npm()
            p4 = vw(p_[:, 0:256], lambda a: a.rearrange("p (h n) -> p h n", h=4))
            for h in range(4):
                S.mm(vw(p4, lambda a: a[:, h, :]), MgT[:, h, :], zt[:, 512 + h * 64:512 + (h + 1) * 64])
            S.copy("act", Q["Y0"][:], p4)
            S.drain_bg()
            S.bg = seqgen(i, d, Q)

        def seqgen(i, d, Q):
            Ad = A[d]
            rhoT = Q["rhoT"]
            for blk in ((0, 1) if d == 0 else (1, 0)):
                p0 = blk * 64
                ps_ = slice(p0, p0 + 64)
                U = Q["U"][blk]
                for h in range(4):
                    S.mm(pU[:, h, :], Q["W1T"][:, h, :], Ad[:, h, :])
                yield
                S.stt("dve", U[ps_, :, :], pU[ps_, :, :], -1.0, Q["W2"][ps_, :, :], ALU.mult, ALU.subtract)
                yield
                for h in range(4):
                    S.mm(pY[:, h, :], rhoT[:, h, :], Ad[:, h, :], start=True, stop=False)
                    S.mm(pY[:, h, :], Q["MbT"][:, h, :], U[:, h, :], start=False, stop=True)
                yield
                for h in range(4):
                    S.mm(pA[:, h, :], Q["betp"][:, h * 64:(h + 1) * 64], U[:, h, :], start=True, stop=False)
                    S.mm(pA[:, h, :], Q["gampm"][:, blk, h * 64:(h + 1) * 64], Q["vq"][:, h * 64:(h + 1) * 64], start=False, stop=True)
                yield
                S.tt("pool", Ad[:], Ad[:], bh(vw(Q["PC"][:], lambda a: a[:, :, blk])), ALU.mult)
                yield
                S.tt("dve", Ad[:], Ad[:], pA[:], ALU.add)
                yield
                S.tt("dve", h4(Yd[d][ps_, i, :]), pY[ps_, :, :], Q["Y0"][ps_, :, :], ALU.add)
                yield

        fwd_order = list(range(NTB))
        rev_order = [1, 0] + list(range(NTB - 1, 1, -1))
        nu = 0
        for s in range(NTB):
            if not (CTX.get("own_only", False) and fwd_order[s] >= 10):
                unit(fwd_order[s], 0, sq[nu % NSQ], True)
                nu += 1
            unit(rev_order[s], 1, sq[nu % NSQ], False)
            nu += 1
        S.drain_bg()
        y = T256("ry")
        yc = T256("ryc")
        ot = [T256("rot%d" % i) for i in range(2)]
        rst = S.sb([128, 24], name="rst")
        for i in range(NTB):
            if CTX.get("own_only", False) and i >= 10:
                continue
            S.tt("dve", y[:], Yd[0][:, i, :], Yd[1][:, i, :], ALU.add)
            S.reduce("dve", rst[:, 0:4], h4(y[:]), ALU.add)
            S.ts("dve", rst[:, 4:8], rst[:, 0:4], 1.0 / 64, None, ALU.mult)
            S.tt("dve", h4(yc[:]), h4(y[:]), bh(rst[:, 4:8]), ALU.subtract)
            S.tt("pool", y[:], yc[:], yc[:], ALU.mult)
            S.reduce("dve", rst[:, 8:12], h4(y[:]), ALU.add)
            S.ts("dve", rst[:, 12:16], rst[:, 8:12], 1.0 / 64, GN_EPS, ALU.mult, ALU.add)
            S.act(rst[:, 16:20], rst[:, 12:16], AF.Sqrt)
            S.recip(rst[:, 20:24], rst[:, 16:20])
            S.tt("dve", h4(yc[:]), h4(yc[:]), bh(rst[:, 20:24]), ALU.mult)
            S.tt("pool", yc[:], yc[:], gng_b, ALU.mult)
            S.tt("pool", yc[:], yc[:], gnb_b, ALU.add)
            S.tt("dve", rst[:, 0:4], bon[:, i, 0:4], bon[:, i, 4:8], ALU.add)
            S.tt("dve", h4(y[:]), h4(vS[:, i, :]), bh(rst[:, 0:4]), ALU.mult)
            S.tt("pool", yc[:], yc[:], y[:], ALU.add)
            o_ = ot[i % 2]
            S.tt("dve", o_[:], yc[:], gS[:, i, :], ALU.mult)
            S.dma(("sp", "act")[i % 2], dv(Or, lambda a: a[i * 128:(i + 1) * 128, :]), o_[:], is_output=True)
        barrier(S)
        print("Brwkv ninstr", S.ninstr)
    return nc


def rwkv_consts():
    idx = np.arange(128)
    blk = idx // 64
    same = blk[:, None] == blk[None, :]
    lt = idx[:, None] < idx[None, :]
    gt = idx[:, None] > idx[None, :]
    eye = np.eye(128, dtype=bool)
    c = np.stack([same & lt, same & gt, same & (lt | eye), same & (gt | eye), same, eye], 1).astype(np.float32)
    return np.ascontiguousarray(c.reshape(128, 6 * 128))


def rwkv_cols(g):
    c = []
    for j in range(3):
        c += list(range(512 * j + 256 * g, 512 * j + 256 * g + 256))
    c += list(range(1536, 1792))
    return np.array(c)


def run_Brwkv(Pfull, shift_prev, shift_next, decay_w0, decay_up, iclr_a0, iclr_up, gate_up, k_k, k_a, r_k, gn_g, gn_b):
    nc = get_nc("Brwkv", build_Brwkv)
    consts = rwkv_consts()
    maps = []
    for core in range(NCORES):
        b, g = core // 2, core % 2
        cols = rwkv_cols(g)
        hs = slice(256 * g, 256 * g + 256)
        w0a0 = np.concatenate([decay_w0[0, hs], iclr_a0[0, hs], decay_w0[1, hs], iclr_a0[1, hs]]).reshape(1, -1)
        wup = np.concatenate([decay_up[0][:, hs], decay_up[1][:, hs]], 1)
        aup = np.concatenate([iclr_up[0][:, hs], iclr_up[1][:, hs]], 1)
        vecs = np.concatenate([k_k[hs], k_a[hs], r_k[hs], gn_g[hs], gn_b[hs]]).reshape(1, -1)
        maps.append(dict(Pr=np.ascontiguousarray(Pfull[b][:, cols]), mup=np.ascontiguousarray(shift_prev[cols].reshape(1, -1)),
                         mun=np.ascontiguousarray(shift_next[cols].reshape(1, -1)), w0a0=np.ascontiguousarray(w0a0),
                         wup=np.ascontiguousarray(wup), aup=np.ascontiguousarray(aup), gup=np.ascontiguousarray(gate_up[:, hs]),
                         vecs=np.ascontiguousarray(vecs), consts=consts))
    res = run_bass_kernel_spmd(nc, maps, core_ids=list(range(NCORES)))
    return [r["Or"] for r in res.results]


NEXP = 32
TG = [(0, 512), (512, 512), (1024, 128)]


def build_C(final):
    nc = CTX["nc"]
    io = CTX["io"]
    xin = io["xin"]
    Oin = io["Oin"]
    mod = io["mod"]
    w_out = io["w_out"]
    g2 = io["g2"]
    wr = io["wr"]
    br = io["br"]
    w1 = io["w1"]
    w3 = io["w3"]
    w2 = io["w2"]
    gf = io["gf"]
    ident = io["ident"]
    xout = io["xout"]
    with ExitStack() as st:
        S = CTX["S"]
        S.stack = st
        idt = S.sb([128, 128], name="idt")
        S.dma("sp", idt[:], ident)
        idb = S.sb([128, 128], BF16, name="idb")
        S.copy("dve", idb[:], idt[:])
        xres = S.sb([128, NT, D], name="xres")
        for i in range(NT):
            S.dma(("sp", "act")[i % 2], xres.k(i, (slice(None), i, slice(None))), dv(xin, lambda a: a[i * 128:(i + 1) * 128, :]))
        def modb(r, j, name, stack, q="sp"):
            t = S.sb([128, D], name=name, stack=stack)
            S.dma(q, t[:], dv(mod, lambda a: a[r:r + 1, j * D:(j + 1) * D].partition_broadcast(128)))
            return t
        def setof(i):
            return 1 if i == 0 else 0
        with ExitStack() as st1:
            gate1 = [modb(r, 2, "gate1_%d" % r, st1, ("sp", "act")[r]) for r in range(2)]
            OT = S.sb([128, NT, 16, 128], BF16, name="OT", stack=st1)
            ot_f = [S.sb([128, D], name="ot_f%d" % i, stack=st1) for i in range(2)]
            ot_b = S.sb([128, D], BF16, name="ot_b", stack=st1)
            ptr = [S.ps([128, 4, 128], BF16, name="ptrC%d" % i, stack=st1) for i in range(2)]
            for i in range(NT):
                o_ = ot_f[i % 2]
                S.dma(("sp", "pool")[i % 2], o_[:], dv(Oin, lambda a: a[i * 128:(i + 1) * 128, :]))
                S.copy("pool", ot_b[:], o_[:])
                for q in range(4):
                    p_ = ptr[q % 2]
                    for c4 in range(4):
                        c = q * 4 + c4
                        S.tr(p_[:, c4, :], ot_b[:, c * 128:(c + 1) * 128], idb[:])
                    S.copy(("dve", "act")[q % 2], OT[:, i, q * 4:q * 4 + 4, :], p_[:])
            wst = [S.sb([128, 4, 512], name="wstC%d" % i, stack=st1) for i in range(3)]
            wbf = [S.sb([128, 16, 512], BF16, name="wbfC%d" % i, stack=st1) for i in range(2)]
            po = [S.ps([128, 512], name="poC%d" % i, stack=st1) for i in range(3)]
            tmp = [S.sb([128, 512], name="tmpC%d" % i, stack=st1) for i in range(2)]
            k = 0
            n = 0
            for j in range(4):
                wb = wbf[j % 2]
                for q in range(4):
                    ws = wst[k % 3]
                    S.dma(("sp", "pool")[k % 2], ws[:],
                          dv(w_out, lambda a: a[q * 512:(q + 1) * 512, j * 512:(j + 1) * 512].rearrange("(c p) n -> p c n", p=128)))
                    S.copy(rr(k), wb.k(q, (slice(None), slice(q * 4, q * 4 + 4), slice(None))), ws[:])
                    k += 1
                for i in range(NT):
                    p_ = po[n % 3]
                    t_ = tmp[n % 2]
                    n += 1
                    for c in range(16):
                        S.mm(p_[:], OT[:, i, c, :], wb.k(c // 4, (slice(None), c, slice(None))), start=(c == 0), stop=(c == 15))
                    S.tt("dve", t_[:], p_[:], gate1[setof(i)][:, j * 512:(j + 1) * 512], ALU.mult)
                    xs = xres.k(i, (slice(None), i, slice(j * 512, (j + 1) * 512)))
                    S.tt("pool", xs, xs, t_[:], ALU.add)
            barrier(S)
        DC = 99
        h2T = S.sb([128, NT, 16, 128], BF16, name="h2T")
        comb = S.sb([128, NT, 32], name="comb")
        gate2 = [modb(r, 5, "gate2_%d" % r, st, ("sp", "act")[r]) for r in range(2)]
        with ExitStack() as st2:
            g2b = S.sb([128, D], name="g2b", stack=st2)
            S.dma("pool", g2b[:], dv(g2, lambda a: a.partition_broadcast(128)))
            gs = []
            sh = []
            for r in range(2):
                sh.append(modb(r, 3, "sh2_%d" % r, st2, "sp"))
                sc_ = modb(r, 4, "sc2_%d" % r, st2, "act")
                S.stt("dve", sc_[:], sc_[:], 1.0, g2b[:], ALU.add, ALU.mult)
                gs.append(sc_)
            wr_s = S.sb([128, 16, 36], name="wr_s", stack=st2)
            S.dma("sp", wr_s[:], dv(wr, lambda a: a.rearrange("(c p) n -> p c n", p=128)))
            br_b = S.sb([128, 36], name="br_b", stack=st2)
            S.dma("act", br_b[:], dv(br, lambda a: a.partition_broadcast(128)))
            junk = S.sb([128, D], name="junkC", stack=st2)
            h2 = S.sb([128, D], name="h2", stack=st2)
            h2T32 = S.sb([128, 16, 128], name="h2T32", stack=st2)
            stat = S.sb([128, NT, 4], name="statC", stack=st2)
            ptf = [S.ps([128, 4, 128], name="ptf%d" % i, stack=st2) for i in range(2)]
            plog = S.ps([128, 36], name="plog", stack=st2)
            rt = [S.sb([128, 128], name="rt%d" % i, stack=st2) for i in range(2)]
            for i in range(NT if DC >= 2 else 0):
                r = setof(i)
                xi = xres.k(i, (slice(None), i, slice(None)))
                S.act(junk[:], xi, AF.Square, accum=stat[:, i, 0:1])
                rstd_of(S, stat[:, i, 1:2], stat[:, i, 0:1], D, 1e-6, (stat[:, i, 2:3], stat[:, i, 3:4]))
                S.stt("dve", junk[:], xi, stat[:, i, 1:2], gs[r][:], ALU.mult, ALU.mult)
                S.tt("pool", h2[:], junk[:], sh[r][:], ALU.add)
                C2 = 99
                if C2 < 1:
                    continue
                for q in range(4):
                    p_ = ptf[q % 2]
                    for c4 in range(4):
                        c = q * 4 + c4
                        S.tr(p_[:, c4, :], h2[:, c * 128:(c + 1) * 128], idt[:])
                    S.copy("act", h2T32[:, q * 4:q * 4 + 4, :], p_[:])
                    S.copy("dve", h2T[:, i, q * 4:q * 4 + 4, :], p_[:])
                if C2 < 2:
                    continue
                for c in range(16):
                    S.mm(plog[:], h2T32[:, c, :], wr_s[:, c, :], start=(c == 0), stop=(c == 15))
                R = rt[i % 2]
                S.tt("dve", R[:, 0:36], plog[:], br_b[:], ALU.add)
                if C2 < 3:
                    continue
                S.reduce("dve", R[:, 36:37], R[:, 0:4], ALU.max)
                S.ts("dve", R[:, 37:38], R[:, 36:37], -1.0, None, ALU.mult)
                S.ts("dve", R[:, 40:44], R[:, 0:4], R[:, 36:37], None, ALU.is_equal)
                S.act(R[:, 44:48], R[:, 0:4], AF.Exp, bias=R[:, 37:38], accum=R[:, 38:39])
                S.recip(R[:, 39:40], R[:, 38:39])
                S.tt("dve", vw(R[:, 48:80], lambda a: a.rearrange("p (g e) -> p g e", g=4)),
                     vw(R[:, 4:36], lambda a: a.rearrange("p (g e) -> p g e", g=4)),
                     vw(R[:, 40:44], lambda a: a.unsqueeze(2).to_broadcast([128, 4, 8])), ALU.mult)
                S.reduce("dve", R[:, 80:88], vw(R[:, 48:80], lambda a: a.rearrange("p (g e) -> p e g", g=4)), ALU.add)
                S.reduce("dve", R[:, 88:89], R[:, 80:88], ALU.max)
                S.ts("dve", R[:, 89:97], R[:, 80:88], R[:, 88:89], None, ALU.is_equal)
                S.stt("dve", R[:, 97:105], R[:, 89:97], -1e30, R[:, 80:88], ALU.mult, ALU.add)
                S.reduce("dve", R[:, 105:106], R[:, 97:105], ALU.max)
                S.ts("dve", R[:, 106:114], R[:, 97:105], R[:, 105:106], None, ALU.is_equal)
                S.tt("dve", R[:, 114:115], R[:, 105:106], R[:, 88:89], ALU.subtract)
                S.act(R[:, 115:116], R[:, 114:115], AF.Exp)
                S.ts("dve", R[:, 116:117], R[:, 115:116], 1.0, None, ALU.add)
                S.recip(R[:, 117:118], R[:, 116:117])
                S.tt("dve", R[:, 118:119], R[:, 115:116], R[:, 117:118], ALU.mult)
                S.ts("dve", R[:, 119:127], R[:, 89:97], R[:, 117:118], None, ALU.mult)
                S.stt("dve", R[:, 119:127], R[:, 106:114], R[:, 118:119], R[:, 119:127], ALU.mult, ALU.add)
                S.tt("dve", vw(R[:, 48:80], lambda a: a.rearrange("p (g e) -> p g e", g=4)),
                     vw(R[:, 40:44], lambda a: a.unsqueeze(2).to_broadcast([128, 4, 8])),
                     vw(R[:, 119:127], lambda a: a.unsqueeze(1).to_broadcast([128, 4, 8])), ALU.mult)
                S.ts("dve", comb[:, i, :], R[:, 48:80], R[:, 39:40], None, ALU.mult)
            barrier(S)
        stg = [S.sb([128, 8, 256], name="stg%d" % i) for i in range(3)]
        w13b = S.sb([128, 2, 16, 256], BF16, name="w13b")
        w2b = [S.sb([128, 2, 2048], BF16, name="w2b%d" % i) for i in range(2)]
        actT = [S.sb([128, 2, TOK], BF16, name="actT%d" % i) for i in range(2)]
        sil = [S.sb([128, 512], name="sil%d" % i) for i in range(2)]
        tmpe = [S.sb([128, 512], name="tmpe%d" % i) for i in range(2)]
        pab = [S.ps([128, 512], name="pab%d" % i) for i in range(4)]
        pdn = [S.ps([128, 512], name="pdn%d" % i) for i in range(3)]
        ks = 0
        nab = 0
        nd = 0
        def load_expert(e):
            nonlocal ks
            for wi, wsrc in enumerate((w1, w3)):
                for hf in range(2):
                    s_ = stg[ks % 3]
                    S.dma("sp", s_[:],
                          dv(wsrc, lambda a: a[e, hf * 1024:(hf + 1) * 1024, :].rearrange("(c p) n -> p c n", p=128)))
                    S.copy(rr(ks), w13b.k((wi, hf), (slice(None), wi, slice(hf * 8, hf * 8 + 8), slice(None))), s_[:])
                    ks += 1
            w2n = w2b[e % 2]
            for hc in range(2):
                s_ = stg[ks % 3]
                S.dma("sp", vw(s_[:], lambda a: a.rearrange("p c n -> p (c n)")),
                      dv(w2, lambda a: a[e, hc * 128:(hc + 1) * 128, :]))
                S.copy(rr(ks), w2n.k(hc, (slice(None), hc, slice(None))), vw(s_[:], lambda a: a.rearrange("p c n -> p (c n)")))
                ks += 1
        load_expert(0)
        for e in range(NEXP):
            w2_ = w2b[e % 2]
            aT = actT[e % 2]
            for hc in range(2):
                for (t0, tn) in TG:
                    pa = pab[nab % 4]
                    pb = pab[(nab + 1) % 4]
                    nab += 2
                    i0 = t0 // 128
                    ni = tn // 128
                    rhs_ = lambda c: vw(h2T[:, i0:i0 + ni, c, :], lambda a: a)
                    for c in range(16):
                        S.mm(vw(pa[:, 0:tn], lambda a: a.rearrange("p (i t) -> p i t", i=ni)),
                             w13b.k((0, c // 8), (slice(None), 0, c, slice(hc * 128, (hc + 1) * 128))), rhs_(c),
                             start=(c == 0), stop=(c == 15))
                    for c in range(16):
                        S.mm(vw(pb[:, 0:tn], lambda a: a.rearrange("p (i t) -> p i t", i=ni)),
                             w13b.k((1, c // 8), (slice(None), 1, c, slice(hc * 128, (hc + 1) * 128))), rhs_(c),
                             start=(c == 0), stop=(c == 15))
                    s_ = sil[(nab // 2) % 2]
                    S.act(s_[:, 0:tn], pa[:, 0:tn], AF.Silu)
                    S.tt("dve", aT[:, hc, t0:t0 + tn], s_[:, 0:tn], pb[:, 0:tn], ALU.mult)
            if e + 1 < NEXP:
                load_expert(e + 1)
            for i in range(NT):
                for j in range(4):
                    p_ = pdn[nd % 3]
                    t_ = tmpe[nd % 2]
                    nd += 1
                    for hc in range(2):
                        S.mm(p_[:], aT[:, hc, i * 128:(i + 1) * 128], w2_.k(hc, (slice(None), hc, slice(j * 512, (j + 1) * 512))),
                             start=(hc == 0), stop=(hc == 1))
                    S.stt("dve", t_[:], p_[:], comb[:, i, e:e + 1], gate2[setof(i)][:, j * 512:(j + 1) * 512], ALU.mult, ALU.mult)
                    xs = xres.k(i, (slice(None), i, slice(j * 512, (j + 1) * 512)))
                    S.tt("pool", xs, xs, t_[:], ALU.add)
        if final:
            gfb = S.sb([128, 512], name="gfb")
            fstat = S.sb([128, NT, 4], name="fstat")
            for i in range(NT):
                xi = xres.k(i, (slice(None), i, slice(None)))
                for j in range(4):
                    S.act(sil[j % 2][:], xres.k(i, (slice(None), i, slice(j * 512, (j + 1) * 512))), AF.Square,
                          accum=fstat[:, i, j:j + 1])
            fs2 = S.sb([128, NT, 4], name="fs2")
            for i in range(NT):
                S.reduce("dve", fs2[:, i, 0:1], fstat[:, i, :], ALU.add)
                rstd_of(S, fs2[:, i, 1:2], fs2[:, i, 0:1], D, 1e-6, (fs2[:, i, 2:3], fs2[:, i, 3:4]))
            for j in range(4):
                S.dma("sp", gfb[:], dv(gf, lambda a: a[:, j * 512:(j + 1) * 512].partition_broadcast(128)))
                for i in range(NT):
                    xs = xres.k(i, (slice(None), i, slice(j * 512, (j + 1) * 512)))
                    S.stt("dve", xs, xs, fs2[:, i, 1:2], gfb[:], ALU.mult, ALU.mult)
        for i in range(NT):
            S.dma(("sp", "act")[i % 2], dv(xout, lambda a: a[i * 128:(i + 1) * 128, :]), xres.k(i, (slice(None), i, slice(None))), is_output=True)
        barrier(S)
        print("C ninstr", S.ninstr)
    return nc


def run_C(xtok, Otok, mods, w_out_l, g2_l, gw, gb, ew, eb, w1_l, w3_l, w2_l, gf, final):
    nc = get_nc("C%d" % int(final), lambda: build_C(final))
    ident = np.eye(128, dtype=np.float32)
    wr = np.ascontiguousarray(np.concatenate([gw, ew], 1))
    br = np.ascontiguousarray(np.concatenate([gb, eb]).reshape(1, -1))
    maps = []
    for core in range(NCORES):
        maps.append(dict(xin=xtok[core], Oin=Otok[core], mod=mods[core], w_out=w_out_l, g2=g2_l.reshape(1, -1), wr=wr, br=br,
                         w1=w1_l, w3=w3_l, w2=w2_l, gf=gf.reshape(1, -1), ident=ident))
    res = run_bass_kernel_spmd(nc, maps, core_ids=list(range(NCORES)))
    return [r["xout"] for r in res.results]


def dcopy(S, dst, src, rows, n=0, chunk=256):
    qs = ("sp", "act", "pool")
    for k, r0 in enumerate(range(0, rows, chunk)):
        r1 = min(rows, r0 + chunk)
        S.dma(qs[(n + k) % 3], dv(dst, lambda a: a[r0:r1]), dv(src, lambda a: a[r0:r1]))


def rows_v(v):
    return [(128 * v, 128, 0), (256 + 1024 * v, 1024, 128)]


def build_fused():
    nc = bass.Bass("TRN2", target_bir_lowering=False)
    E = {}
    def ein(name, shape):
        E[name] = dram_in(nc, name, shape)
        return E[name]
    def scr(name, shape):
        return V(nc.dram_tensor(name, list(shape), F32).ap(), ("dram_" + name,))
    ein("x_seq", [TB, D])
    ein("cT", [128, 32])
    ein("ident", [128, 128])
    ein("consts", [128, 768])
    ein("cosG", [2048, 64]); ein("sinG", [2048, 64]); ein("cosM", [2048, 32]); ein("sinM", [2048, 32])
    ein("gf", [1, D])
    for l in range(2):
        ein("mod_w%d" % l, [D, 6 * D]); ein("mod_b%d" % l, [1, 6 * D]); ein("g1_%d" % l, [1, D]); ein("w_in%d" % l, [D, DIN])
        ein("w_out%d" % l, [D, D]); ein("g2_%d" % l, [1, D]); ein("wr%d" % l, [D, 36]); ein("br%d" % l, [1, 36])
        ein("w1_%d" % l, [NEXP, D, 256]); ein("w3_%d" % l, [NEXP, D, 256]); ein("w2_%d" % l, [NEXP, 256, D])
        ein("gq%d" % l, [1, 128]); ein("gk%d" % l, [1, 128]); ein("gmq%d" % l, [1, 384]); ein("gmkv%d" % l, [1, 256])
        for g in range(2):
            t = "%d%d" % (l, g)
            ein("wuq" + t, [384, 384]); ein("wukv" + t, [256, 512])
            ein("mup" + t, [1, 1024]); ein("mun" + t, [1, 1024]); ein("w0a0" + t, [1, 1024])
            ein("wup" + t, [64, 512]); ein("aup" + t, [64, 512]); ein("gup" + t, [128, 256]); ein("vecs" + t, [1, 1280])
    xout = dram_out(nc, "xout", [TOK, D])
    xin_v = scr("xin_v", [TOK, D]); P_v = scr("P_v", [TOK, DIN]); P_scr = scr("P_scr", [TB, DIN])
    Pa_scr = scr("Pa_scr", [TB, PA_W]); Pr_scr = scr("Pr_scr", [TB, 1024]); Oa_scr = scr("Oa_scr", [TB, 768])
    Or_scr = scr("Or_scr", [TB, 256]); O_scr = scr("O_scr", [TB, D]); Oin_v = scr("Oin_v", [TOK, D])
    xout_v = scr("xout_v", [TOK, D]); x1_scr = scr("x1_scr", [TB, D])
    mod_scr = [scr("mod_scr%d" % l, [2, 6 * D]) for l in range(2)]
    with ExitStack() as st0:
        S = Sched(nc, st0)
        CTX["nc"] = nc
        CTX["S"] = S
        def cs(v_, r0, r1, c0, c1):
            return dv(v_, lambda a: a[r0:r1, c0:c1])
        for l in range(2):
            xsrc = E["x_seq"] if l == 0 else x1_scr
            for v in range(2):
                S.prefix = "A%d%d_" % (l, v)
                for (s0, n, d0) in rows_v(v):
                    dcopy(S, cs(xin_v, d0, d0 + n, 0, D), cs(xsrc, s0, s0 + n, 0, D), n)
                CTX["do_mod"] = (v == 0)
                CTX["io"] = dict(xin=xin_v, cT=E["cT"], mod_w=E["mod_w%d" % l], mod_b=E["mod_b%d" % l], g1=E["g1_%d" % l],
                                 w_in=E["w_in%d" % l], ident=E["ident"], P=P_v, mod=mod_scr[l])
                build_A()
                for (s0, n, d0) in rows_v(v):
                    dcopy(S, cs(P_scr, s0, s0 + n, 0, DIN), cs(P_v, d0, d0 + n, 0, DIN), n)
            CTX["own_only"] = (l == 1)
            for g in range(2):
                t = "%d%d" % (l, g)
                S.prefix = "R%s_" % t
                segs = [(256 * g, 256), (512 + 256 * g, 256), (1024 + 256 * g, 256), (1536, 256)]
                c0 = 0
                for k, (sc0, w) in enumerate(segs):
                    dcopy(S, cs(Pr_scr, 0, TB, c0, c0 + w), cs(P_scr, 0, TB, sc0, sc0 + w), TB, n=k)
                    c0 += w
                CTX["io"] = dict(Pr=Pr_scr, mup=E["mup" + t], mun=E["mun" + t], w0a0=E["w0a0" + t], wup=E["wup" + t],
                                 aup=E["aup" + t], gup=E["gup" + t], vecs=E["vecs" + t], consts=E["consts"], Or=Or_scr)
                build_Brwkv()
                dcopy(S, cs(O_scr, 0, TB, 256 * g, 256 * g + 256), Or_scr, TB)
                S.prefix = "T%s_" % t
                q0 = 1792
                m0 = 1792 + 1536
                segs = [(q0 + 512 * g, 512), (q0 + 1024 + 128 * g, 128), (q0 + 1280 + 128 * g, 128), (m0, 704)]
                c0 = 0
                for k, (sc0, w) in enumerate(segs):
                    dcopy(S, cs(Pa_scr, 0, TB, c0, c0 + w), cs(P_scr, 0, TB, sc0, sc0 + w), TB, n=k)
                    c0 += w
                CTX["io"] = dict(Pa=Pa_scr, gq=E["gq%d" % l], gk=E["gk%d" % l], gmq=E["gmq%d" % l], gmkv=E["gmkv%d" % l],
                                 wuq=E["wuq" + t], wukv=E["wukv" + t], cosG=E["cosG"], sinG=E["sinG"], cosM=E["cosM"],
                                 sinM=E["sinM"], ident=E["ident"], Oa=Oa_scr)
                build_Battn()
                dcopy(S, cs(O_scr, 0, TB, 512 + 512 * g, 512 + 512 * g + 512), cs(Oa_scr, 0, TB, 0, 512), TB)
                dcopy(S, cs(O_scr, 0, TB, 1536 + 256 * g, 1536 + 256 * g + 256), cs(Oa_scr, 0, TB, 512, 768), TB, n=1)
            for v in ((0, 1) if l == 0 else (0,)):
                S.prefix = "C%d%d_" % (l, v)
                for (s0, n, d0) in rows_v(v):
                    dcopy(S, cs(xin_v, d0, d0 + n, 0, D), cs(xsrc, s0, s0 + n, 0, D), n)
                    dcopy(S, cs(Oin_v, d0, d0 + n, 0, D), cs(O_scr, s0, s0 + n, 0, D), n, n=1)
                final = (l == 1)
                CTX["io"] = dict(xin=xin_v, Oin=Oin_v, mod=mod_scr[l], w_out=E["w_out%d" % l], g2=E["g2_%d" % l], wr=E["wr%d" % l],
                                 br=E["br%d" % l], w1=E["w1_%d" % l], w3=E["w3_%d" % l], w2=E["w2_%d" % l], gf=E["gf"],
                                 ident=E["ident"], xout=(xout if final else xout_v))
                build_C(final)
                if not final:
                    for (s0, n, d0) in rows_v(v):
                        dcopy(S, cs(x1_scr, s0, s0 + n, 0, D), cs(xout_v, d0, d0 + n, 0, D), n)
                    barrier(S)
        S.stack = st0
        S.finish()
        print("fused ninstr", S.ninstr)
    return nc


_FUSED = {}


def kernel(x, c, ctx, c_ctx, mod_w, mod_b, norm1_g, norm2_g, w_in, w_out, shift_prev, shift_next,
           decay_w0, decay_up, iclr_a0, iclr_up, gate_up, k_k, k_a, r_k, gn_g, gn_b, q_norm_g, k_norm_g,
           mla_q_norm_g, mla_w_uq, mla_kv_norm_g, mla_w_ukv, router_gw, router_gb, router_ew, router_eb,
           exp_w1, exp_w3, exp_w2, final_norm_g):
    f = lambda a: np.ascontiguousarray(np.asarray(a, dtype=np.float32))
    x, c, ctx, c_ctx = f(x), f(c), f(ctx), f(c_ctx)
    if "nc" not in _FUSED:
        _FUSED["nc"] = build_fused()
    nc = _FUSED["nc"]
    ident = np.eye(128, dtype=np.float32)
    consts = rwkv_consts()
    cG, sG, cM, sM = rope_tables()
    shared = dict(ident=ident, consts=consts, gf=f(final_norm_g).reshape(1, -1))
    for l in range(2):
        shared["mod_w%d" % l] = f(mod_w[l]); shared["mod_b%d" % l] = f(mod_b[l]).reshape(1, -1)
        shared["g1_%d" % l] = f(norm1_g[l]).reshape(1, -1); shared["w_in%d" % l] = f(w_in[l]); shared["w_out%d" % l] = f(w_out[l])
        shared["g2_%d" % l] = f(norm2_g[l]).reshape(1, -1)
        shared["wr%d" % l] = f(np.concatenate([router_gw[l], router_ew[l]], 1))
        shared["br%d" % l] = f(np.concatenate([router_gb[l], router_eb[l]])).reshape(1, -1)
        shared["w1_%d" % l] = f(exp_w1[l]); shared["w3_%d" % l] = f(exp_w3[l]); shared["w2_%d" % l] = f(exp_w2[l])
        shared["gq%d" % l] = f(q_norm_g[l]).reshape(1, -1); shared["gk%d" % l] = f(k_norm_g[l]).reshape(1, -1)
        shared["gmq%d" % l] = f(mla_q_norm_g[l]).reshape(1, -1); shared["gmkv%d" % l] = f(mla_kv_norm_g[l]).reshape(1, -1)
        for g in range(2):
            t = "%d%d" % (l, g)
            shared["wuq" + t] = f(np.concatenate([mla_w_uq[l][:, (2 * g + hh) * 192:(2 * g + hh + 1) * 192] for hh in range(2)], 1))
            shared["wukv" + t] = f(np.concatenate([mla_w_ukv[l][:, (2 * g + hh) * 256:(2 * g + hh + 1) * 256] for hh in range(2)], 1))
            hs = slice(256 * g, 256 * g + 256)
            shared["gup" + t] = f(gate_up[l][:, hs])
            shared["vecs" + t] = f(np.concatenate([k_k[l][hs], k_a[l][hs], r_k[l][hs], gn_g[l][hs], gn_b[l][hs]])).reshape(1, -1)
    maps = []
    for core in range(NCORES):
        b, h = core // 2, core % 2
        m = dict(shared)
        if h == 0:
            m["x_seq"] = f(np.concatenate([ctx[b], x[b]], 0))
            m["cosG"], m["sinG"], m["cosM"], m["sinM"] = cG, sG, cM, sM
        else:
            m["x_seq"] = f(np.concatenate([ctx[b][::-1], x[b][::-1]], 0))
            m["cosG"], m["sinG"], m["cosM"], m["sinM"] = f(cG[::-1]), f(sG[::-1]), f(cM[::-1]), f(sM[::-1])
        m["cT"] = cT_layout(c[b], c_ctx)
        d0, d1 = (0, 1) if h == 0 else (1, 0)
        for l in range(2):
            for g in range(2):
                t = "%d%d" % (l, g)
                cols = rwkv_cols(g)
                hs = slice(256 * g, 256 * g + 256)
                sp, sn = (shift_prev[l], shift_next[l]) if h == 0 else (shift_next[l], shift_prev[l])
                m["mup" + t] = f(np.asarray(sp)[cols]).reshape(1, -1)
                m["mun" + t] = f(np.asarray(sn)[cols]).reshape(1, -1)
                m["w0a0" + t] = f(np.concatenate([decay_w0[l][d0, hs], iclr_a0[l][d0, hs], decay_w0[l][d1, hs], iclr_a0[l][d1, hs]])).reshape(1, -1)
                m["wup" + t] = f(np.concatenate([decay_up[l][d0][:, hs], decay_up[l][d1][:, hs]], 1))
                m["aup" + t] = f(np.concatenate([iclr_up[l][d0][:, hs], iclr_up[l][d1][:, hs]], 1))
        maps.append(m)
    res = run_bass_kernel_spmd(nc, maps, core_ids=list(range(NCORES)))
    out = np.empty((4, 2048, D), np.float32)
    for core in range(NCORES):
        b, h = core // 2, core % 2
        y = res.results[core]["xout"][128:]
        if h == 0:
            out[b, 0:1024] = y
        else:
            out[b, 1024:2048] = y[::-1]
    return out
```
